# Optimizing a Trainium2 kernel written in Bass

```python
import jax, jax.numpy as jnp
from jax import lax
import numpy as np

D_MODEL = 1024
BATCH = 16
SEQ = 2048
DEPTH = 1

MIX_WIDTH = D_MODEL
ATT_WIDTH = MIX_WIDTH // 2
ATT_HEAD_DIM = 64
ATT_HEADS = ATT_WIDTH // ATT_HEAD_DIM
MOBA_BLOCK = 256
MOBA_TOPK = 3
MOBA_QCHUNK = 32
MLSTM_WIDTH = MIX_WIDTH - ATT_WIDTH
MLSTM_HEADS = 4
MLSTM_HEAD_DIM = MLSTM_WIDTH // MLSTM_HEADS
MLSTM_CHUNK = 64
MLSTM_CONV = 4
D_FF = ((8 * D_MODEL // 3 + 127) // 128) * 128
FFN_CONV = 3
PROJ_COLS = 3 * ATT_WIDTH + 4 * MLSTM_WIDTH + 2 * MLSTM_HEADS
EPS = 1e-6

kernel_name = "hymba_moba_mlstm_convffn"


def _proj_splits():
    sizes = [ATT_WIDTH] * 3 + [MLSTM_WIDTH] * 4 + [MLSTM_HEADS] * 2
    return [int(s) for s in np.cumsum(sizes)[:-1]]


def rms_norm(x, g):
    xf = x.astype(jnp.float32)
    y = xf * lax.rsqrt(jnp.mean(xf * xf, axis=-1, keepdims=True) + EPS)
    return (y * g.astype(jnp.float32)).astype(x.dtype)


def head_rms_norm(y, n_heads, g):
    B, S, W = y.shape
    yf = y.astype(jnp.float32).reshape(B, S, n_heads, W // n_heads)
    yf = yf * lax.rsqrt(jnp.mean(yf * yf, axis=-1, keepdims=True) + EPS)
    return (yf.reshape(B, S, W) * g.astype(jnp.float32)).astype(y.dtype)


def causal_dwconv(x, w, b):
    K, C = w.shape
    y = lax.conv_general_dilated(x, w[:, None, :].astype(x.dtype), window_strides=(1,),
                                 padding=((K - 1, 0),), dimension_numbers=('NWC', 'WIO', 'NWC'),
                                 feature_group_count=C)
    return y + b.astype(x.dtype)


def moba_attention(q, k, v):
    B, S, H, Dh = q.shape
    L = MOBA_BLOCK
    nb = -(-S // L)
    pad = ((0, 0), (0, nb * L - S), (0, 0), (0, 0))

    def blocks(a):
        return jnp.pad(a.astype(jnp.float32), pad).reshape(B, nb, L, H, Dh).transpose(0, 3, 1, 2, 4)

    kb, vb = blocks(k), blocks(v)
    k_mean = jnp.mean(kb, axis=3)
    qf = q.astype(jnp.float32).transpose(0, 2, 1, 3) * (Dh ** -0.5)
    nq = S // MOBA_QCHUNK
    q_chunks = qf.reshape(B, H, nq, MOBA_QCHUNK, Dh).transpose(2, 0, 1, 3, 4)
    k_eff = min(MOBA_TOPK, nb)
    b_idx = jnp.arange(B)[:, None, None, None]
    h_idx = jnp.arange(H)[None, :, None, None]

    def one_chunk(args):
        q_c, c = args
        start = c * MOBA_QCHUNK
        q_pos = start + jnp.arange(MOBA_QCHUNK)
        blk = start // L
        gate = jnp.einsum('bhqd,bhnd->bhqn', q_c, k_mean)
        gate = jnp.where(jnp.arange(nb) < blk, gate, -jnp.inf)
        _, sel = lax.top_k(gate, k_eff)
        slot_ok = jnp.arange(k_eff) < blk
        k_sel = kb[b_idx, h_idx, sel]
        v_sel = vb[b_idx, h_idx, sel]
        k_own = lax.dynamic_index_in_dim(kb, blk, axis=2, keepdims=False)
        v_own = lax.dynamic_index_in_dim(vb, blk, axis=2, keepdims=False)
        k_pos = blk * L + jnp.arange(L)
        s_own = jnp.einsum('bhqd,bhkd->bhqk', q_c, k_own)
        s_own = jnp.where(k_pos[None, :] <= q_pos[:, None], s_own, -jnp.inf)
        s_sel = jnp.einsum('bhqd,bhqjkd->bhqjk', q_c, k_sel)
        s_sel = jnp.where(slot_ok[:, None], s_sel, -jnp.inf)
        s = jnp.concatenate([s_own, s_sel.reshape(B, H, MOBA_QCHUNK, k_eff * L)], axis=-1)
        p = jax.nn.softmax(s, axis=-1)
        p_sel = p[..., L:].reshape(B, H, MOBA_QCHUNK, k_eff, L)
        return (jnp.einsum('bhqk,bhkd->bhqd', p[..., :L], v_own)
                + jnp.einsum('bhqjk,bhqjkd->bhqd', p_sel, v_sel))

    out = lax.map(one_chunk, (q_chunks, jnp.arange(nq)))
    return out.transpose(1, 0, 3, 2, 4).reshape(B, S, H * Dh)


def mlstm_chunkwise(q, k, v, i_pre, f_pre):
    B, S, H, D = q.shape
    L = MLSTM_CHUNK
    nc = S // L
    f32 = jnp.float32

    def chunks(a):
        return a.astype(f32).reshape(B, nc, L, H, D).transpose(1, 0, 3, 2, 4)

    def gchunks(a):
        return a.reshape(B, nc, L, H).transpose(1, 0, 3, 2)

    qc = chunks(q) * (D ** -0.5)
    kc, vc = chunks(k), chunks(v)
    ic = gchunks(i_pre.astype(f32))
    lfc = gchunks(jax.nn.log_sigmoid(f_pre.astype(f32)))
    causal = jnp.tril(jnp.ones((L, L), dtype=bool))

    def step(carry, inp):
        C, n, m = carry
        q_, k_, v_, i_, lf_ = inp
        b = jnp.cumsum(lf_, axis=-1)
        g = b + m[..., None]
        dm = b[..., :, None] - b[..., None, :] + i_[..., None, :]
        dm = jnp.where(causal, dm, -jnp.inf)
        m_t = jnp.maximum(g, jnp.max(dm, axis=-1))
        w_inter = jnp.exp(g - m_t)
        w_intra = jnp.exp(dm - m_t[..., None])
        qk = jnp.einsum('bhtd,bhsd->bhts', q_, k_) * w_intra
        num = (w_inter[..., None] * jnp.einsum('bhtd,bhde->bhte', q_, C)
               + jnp.einsum('bhts,bhse->bhte', qk, v_))
        den = w_inter * jnp.einsum('bhtd,bhd->bht', q_, n) + jnp.sum(qk, axis=-1)
        h = num / jnp.maximum(jnp.abs(den), jnp.exp(-m_t))[..., None]
        decay = w_inter[..., -1]
        w_last = w_intra[..., -1, :]
        C_new = decay[..., None, None] * C + jnp.einsum('bhs,bhsd,bhse->bhde', w_last, k_, v_)
        n_new = decay[..., None] * n + jnp.einsum('bhs,bhsd->bhd', w_last, k_)
        return (C_new, n_new, m_t[..., -1]), h

    init = (jnp.zeros((B, H, D, D), f32), jnp.zeros((B, H, D), f32), jnp.zeros((B, H), f32))
    _, hs = lax.scan(step, init, (qc, kc, vc, ic, lfc))
    return hs.transpose(1, 0, 3, 2, 4).reshape(B, S, H * D)


def setup_inputs(seed: int = 0) -> dict:
    key = jax.random.key(seed)
    ks = jax.random.split(key, 18)
    f32 = jnp.float32
    nrm = lambda k, shape: jax.random.normal(k, shape, f32)
    x = nrm(ks[0], (BATCH, SEQ, D_MODEL))
    norm_mix_g = 1.0 + 0.02 * nrm(ks[1], (DEPTH, D_MODEL))
    w_in = nrm(ks[2], (DEPTH, D_MODEL, PROJ_COLS)) * D_MODEL ** -0.5
    i_bias = 0.1 * nrm(ks[3], (DEPTH, MLSTM_HEADS))
    f_bias = jnp.linspace(3.0, 6.0, MLSTM_HEADS, dtype=f32)[None, :] + 0.1 * nrm(ks[4], (DEPTH, MLSTM_HEADS))
    b_gates = jnp.concatenate([i_bias, f_bias], axis=-1)
    mlstm_conv_w = nrm(ks[5], (DEPTH, MLSTM_CONV, 2 * MLSTM_WIDTH)) * MLSTM_CONV ** -0.5
    mlstm_conv_b = 0.01 * nrm(ks[6], (DEPTH, 2 * MLSTM_WIDTH))
    att_out_g = 1.0 + 0.02 * nrm(ks[7], (DEPTH, ATT_WIDTH))
    mlstm_out_g = 1.0 + 0.02 * nrm(ks[8], (DEPTH, MLSTM_WIDTH))
    w_out = nrm(ks[9], (DEPTH, MIX_WIDTH, D_MODEL)) * MIX_WIDTH ** -0.5
    norm_ffn_g = 1.0 + 0.02 * nrm(ks[10], (DEPTH, D_MODEL))
    w_up = nrm(ks[11], (DEPTH, D_MODEL, 2 * D_FF)) * D_MODEL ** -0.5
    ffn_conv_w = nrm(ks[12], (DEPTH, FFN_CONV, 2 * D_FF)) * FFN_CONV ** -0.5
    ffn_conv_b = 0.01 * nrm(ks[13], (DEPTH, 2 * D_FF))
    w_down = nrm(ks[14], (DEPTH, D_FF, D_MODEL)) * D_FF ** -0.5
    norm_final_g = 1.0 + 0.02 * nrm(ks[15], (D_MODEL,))
    return {"x": x, "norm_mix_g": norm_mix_g, "w_in": w_in, "b_gates": b_gates,
            "mlstm_conv_w": mlstm_conv_w, "mlstm_conv_b": mlstm_conv_b, "att_out_g": att_out_g,
            "mlstm_out_g": mlstm_out_g, "w_out": w_out, "norm_ffn_g": norm_ffn_g, "w_up": w_up,
            "ffn_conv_w": ffn_conv_w, "ffn_conv_b": ffn_conv_b, "w_down": w_down,
            "norm_final_g": norm_final_g}


def reference(x, norm_mix_g, w_in, b_gates, mlstm_conv_w, mlstm_conv_b, att_out_g, mlstm_out_g,
              w_out, norm_ffn_g, w_up, ffn_conv_w, ffn_conv_b, w_down, norm_final_g):
    B, S, _ = x.shape
    splits = _proj_splits()
    h = x
    for l in range(DEPTH):
        a = rms_norm(h, norm_mix_g[l])
        u = a @ w_in[l]
        aq, ak, av, mq, mk, mv, mo, mi, mf = jnp.split(u, splits, axis=-1)
        att = moba_attention(aq.reshape(B, S, ATT_HEADS, ATT_HEAD_DIM),
                             ak.reshape(B, S, ATT_HEADS, ATT_HEAD_DIM),
                             av.reshape(B, S, ATT_HEADS, ATT_HEAD_DIM)).astype(h.dtype)
        att = head_rms_norm(att, ATT_HEADS, att_out_g[l])
        mqk = jax.nn.silu(causal_dwconv(jnp.concatenate([mq, mk], axis=-1), mlstm_conv_w[l], mlstm_conv_b[l]))
        mq, mk = jnp.split(mqk, 2, axis=-1)
        gates = jnp.concatenate([mi, mf], axis=-1) + b_gates[l].astype(h.dtype)
        mh = mlstm_chunkwise(mq.reshape(B, S, MLSTM_HEADS, MLSTM_HEAD_DIM),
                             mk.reshape(B, S, MLSTM_HEADS, MLSTM_HEAD_DIM),
                             mv.reshape(B, S, MLSTM_HEADS, MLSTM_HEAD_DIM),
                             gates[..., :MLSTM_HEADS], gates[..., MLSTM_HEADS:]).astype(h.dtype)
        mh = head_rms_norm(mh, MLSTM_HEADS, mlstm_out_g[l]) * jax.nn.sigmoid(mo)
        h = h + jnp.concatenate([att, mh], axis=-1) @ w_out[l]
        f = causal_dwconv(rms_norm(h, norm_ffn_g[l]) @ w_up[l], ffn_conv_w[l], ffn_conv_b[l])
        fg, fv = jnp.split(f, 2, axis=-1)
        h = h + (jax.nn.silu(fg) * fv) @ w_down[l]
    return rms_norm(h, norm_final_g)
```

```python
import numpy as np
import concourse.bass as bass
import concourse.mybir as mybir
from concourse.bass_utils import run_bass_kernel_spmd

F32 = mybir.dt.float32
BF16 = mybir.dt.bfloat16
ALU = mybir.AluOpType
AF = mybir.ActivationFunctionType
AX = mybir.AxisListType

S = 2048
D = 1024
KC = 8
T = 16
NB = 4
DFF = 2816
NFC = 22
PROJ = 3592
NEG = -30000.0
EPS = 1e-6
MD_SCALE = 128.0 ** -0.5

V_G1, V_G2, V_GF, V_GATT, V_GML, V_MCW, V_MCB, V_FCW, V_FCB, V_BG, NVEC = 0, 8, 16, 24, 28, 32, 64, 72, 204, 248, 256
C_ID, C_TRI, C_ONE, NCST = 0, 128, 256, 384


class Res:
    __slots__ = ("name", "w", "r")

    def __init__(self, name):
        self.name = name
        self.w = None
        self.r = {}


class Sem:
    __slots__ = ("h", "n")

    def __init__(self, h):
        self.h = h
        self.n = 0


class K:
    def __init__(self, nc):
        self.nc = nc
        self.engs = {"pe": nc.tensor, "act": nc.scalar, "dve": nc.vector, "pool": nc.gpsimd, "sp": nc.sync}
        self.esem = {e: Sem(nc.alloc_semaphore("e_" + e)) for e in ("pe", "act", "dve", "pool")}
        self.seen = {e: {} for e in self.engs}
        self.nwait = 0
        self.bank_i = 0
        self.half_i = 0

    def res(self, name):
        return Res(name)

    def newsem(self, name):
        return Sem(self.nc.alloc_semaphore(name))

    def _deps(self, eng, reads, writes):
        deps = []
        for r in reads:
            if r.w is not None:
                deps.append(r.w)
        for w in writes:
            if w.w is not None and w.w[2] != eng:
                deps.append(w.w)
            for d in w.r.values():
                if d[2] != eng:
                    deps.append(d)
        return deps

    def _wait(self, eng, deps):
        best = {}
        for sem, val, src in deps:
            if eng == "pe" and src == "pe":
                continue
            key = id(sem)
            if key not in best or best[key][1] < val:
                best[key] = (sem, val)
        seen = self.seen[eng]
        for key, (sem, val) in best.items():
            if seen.get(key, 0) < val:
                self.engs[eng].wait_ge(sem.h, val)
                seen[key] = val
                self.nwait += 1

    def _record(self, tok, reads, writes):
        for r in reads:
            key = id(tok[0])
            if key not in r.r or r.r[key][1] < tok[1]:
                r.r[key] = tok
        for w in writes:
            w.w = tok
            w.r = {}

    def op(self, eng, fn, reads=(), writes=(), inc=True):
        self._wait(eng, self._deps(eng, reads, writes))
        ins = fn(self.engs[eng])
        s = self.esem[eng]
        if inc:
            s.n += 1
            ins.then_inc(s.h, 1)
            tok = (s, s.n, eng)
        else:
            tok = (s, s.n + 1, eng)
        self._record(tok, reads, writes)
        return ins

    def dma(self, q, out, in_, sem, reads=(), writes=(), **kw):
        qn = "dma_" + q
        self._wait(q, self._deps(qn, reads, writes))
        ins = self.engs[q].dma_start(out=out, in_=in_, **kw)
        sem.n += 16
        ins.then_inc(sem.h, 16)
        self._record((sem, sem.n, qn), reads, writes)
        return ins

    def bank(self):
        b = self.bank_i
        self.bank_i = (self.bank_i + 1) % 8
        return b

    def half(self):
        b = self.half_i * 4
        self.half_i ^= 1
        self.bank_i = (b + 4) % 8
        return b

    def barrier(self):
        self.nc.all_engine_barrier()


def build(debug=False, nseq=2, stages=99):
    nc = bass.Bass("TRN2", target_bir_lowering=False)
    k = K(nc)
    dbg = {}

    xT = nc.dram_tensor("xT", [2, D, S], F32, kind="ExternalInput").ap()
    w_in = nc.dram_tensor("w_in", [D, PROJ], F32, kind="ExternalInput").ap()
    w_out = nc.dram_tensor("w_out", [D, D], F32, kind="ExternalInput").ap()
    w_up = nc.dram_tensor("w_up", [D, 2 * DFF], F32, kind="ExternalInput").ap()
    w_down = nc.dram_tensor("w_down", [DFF, D], F32, kind="ExternalInput").ap()
    vecs_d = nc.dram_tensor("vecs", [128, NVEC], F32, kind="ExternalInput").ap()
    cstf_d = nc.dram_tensor("cstf", [128, NCST], F32, kind="ExternalInput").ap()
    sel_d = nc.dram_tensor("sel", [128, 64 * 128], F32, kind="ExternalInput").ap()
    outT = nc.dram_tensor("outT", [2, D, S], F32, kind="ExternalOutput").ap()

    DTB = {F32: 4, BF16: 2}
    arena = {"ptr": ((nc.sbuf_base + 63) // 64) * 64, "top": nc.sbuf_top}

    def A(name, shape, dt, at=None):
        nb = DTB[dt]
        for d_ in shape[1:]:
            nb *= d_
        off = arena["ptr"] if at is None else at
        if at is None:
            arena["ptr"] = ((off + nb + 63) // 64) * 64
            assert arena["ptr"] <= arena["top"], "SBUF arena overflow at %s: %d > %d" % (name, arena["ptr"], arena["top"])
        return nc.alloc_sbuf_tensor_at("s_" + name, shape, dt, offset=off)

    ps = nc.alloc_psum_tensor("ps", [128, 8, 512], F32)
    pb = [k.res("pb%d" % i) for i in range(8)]
    ps_flat = ps[:, :, :].rearrange("p a b -> p (a b)")

    vecs = A("vecs", [128, NVEC], F32)
    cstf = A("cstf", [128, NCST], F32)
    cstb = A("cstb", [128, NCST], BF16)
    maskU = A("maskU", [128, 128], F32)
    tribias = A("tribias", [128, 128], BF16)
    epsc = A("epsc", [128, 1], F32)
    onec = A("onec", [128, 1], F32)
    r_const = k.res("const")
    dsem = k.newsem("d_const")
    osem = [k.newsem("d_out%d" % kc) for kc in range(KC)]
    gsem = k.newsem("d_dbg")
    xsem = [k.newsem("d_x%d" % kc) for kc in range(KC)]

    hT_r = [[k.res("hT%d_%d" % (kc, tb)) for tb in range(NB)] for kc in range(KC)]
    aT = A("aT", [128, KC, S], BF16)
    aT_r = [k.res("aT%d" % tb) for tb in range(NB)]
    MIXT_OFF = arena["ptr"]
    mixT = A("mixT", [128, KC, S], BF16)
    mixT_r = [k.res("mixT%d" % t) for t in range(T)]
    NW = 3
    wr = [A("wr%d" % i, [128, KC, 512], BF16) for i in range(NW)]
    wr_r = [k.res("wr%d" % i) for i in range(NW)]
    wsem = [k.newsem("d_w%d" % i) for i in range(NW)]
    wri = [0]

    def wslot():
        i = wri[0]
        wri[0] = (i + 1) % NW
        return i

    def load_w512(src_cols_ap):
        i = wslot()
        n = src_cols_ap.shape[1]
        k.dma("pool", out=wr[i][:, :, 0:n], in_=src_cols_ap.rearrange("(kc p) n -> p kc n", p=128),
              sem=wsem[i], writes=[wr_r[i]])
        return i

    ARENA0 = arena["ptr"]
    hT = A("hT", [128, KC, S], F32)
    ARENA1 = arena["ptr"]

    def phase_end():
        if debug and gsem.n:
            k.engs["sp"].wait_ge(gsem.h, gsem.n)
        k.barrier()

    k.dma("sp", out=vecs[:], in_=vecs_d, sem=dsem, writes=[r_const])
    k.dma("sp", out=cstf[:], in_=cstf_d, sem=dsem, writes=[r_const])
    k.op("dve", lambda e: e.tensor_copy(out=cstb[:], in_=cstf[:]), reads=[r_const], writes=[r_const])
    k.op("dve", lambda e: e.tensor_scalar(out=maskU[:], in0=cstf[:, C_TRI:C_TRI + 128], scalar1=MD_SCALE,
                                          scalar2=None, op0=ALU.mult), reads=[r_const], writes=[r_const])
    k.op("dve", lambda e: e.tensor_scalar(out=tribias[:], in0=cstf[:, C_TRI:C_TRI + 128], scalar1=-1.0,
                                          scalar2=-NEG, op0=ALU.add, op1=ALU.mult), reads=[r_const], writes=[r_const])
    k.op("dve", lambda e: e.memset(epsc[:], EPS), writes=[r_const])
    k.op("dve", lambda e: e.memset(onec[:], 1.0), writes=[r_const])
    ident_b = cstb[:, C_ID:C_ID + 128]
    ident_f = cstf[:, C_ID:C_ID + 128]
    tri_b = cstb[:, C_TRI:C_TRI + 128]
    tri_f = cstf[:, C_TRI:C_TRI + 128]
    ones_b = cstb[:, C_ONE:C_ONE + 128]
    ones_f = cstf[:, C_ONE:C_ONE + 128]

    def vcol(c):
        return vecs[:, c:c + 1]

    def dump(name, ap, reads):
        if not debug:
            return
        t = nc.dram_tensor(name, list(ap.shape), ap.dtype, kind="ExternalOutput").ap()
        k.dma("sp", out=t, in_=ap, sem=gsem, reads=reads)
        dbg[name] = t

    def tsl(tb):
        return slice(tb * 512, (tb + 1) * 512)

    def norm_phase(src, src_r, gcol0, dst_fn, sq, sq_r, rstd, rstd_r, kc_outer=False):
        b0 = k.half()
        for kc in range(KC):
            i = kc % 2
            k.op("act", lambda e: e.activation(out=sq[i][:], in_=src[:, kc, :], func=AF.Square),
                 reads=src_r[kc], writes=[sq_r[i]])
            for tb in range(NB):
                k.op("pe", lambda e: e.matmul(out=ps[:, b0 + tb, :], lhsT=ones_b, rhs=sq[i][:, tsl(tb)],
                                              start=(kc == 0), stop=(kc == KC - 1)),
                     reads=[sq_r[i], r_const], writes=[pb[b0 + tb]], inc=(tb == NB - 1))
        for tb in range(NB):
            k.op("act", lambda e: e.activation(out=rstd[:, tsl(tb)], in_=ps[:, b0 + tb, :], func=AF.Ln,
                                               bias=epsc[:, 0:1], scale=1.0 / D),
                 reads=[pb[b0 + tb], r_const], writes=[rstd_r[tb]])
            k.op("act", lambda e: e.activation(out=rstd[:, tsl(tb)], in_=rstd[:, tsl(tb)], func=AF.Exp, scale=-0.5),
                 reads=[rstd_r[tb]], writes=[rstd_r[tb]])
        if kc_outer:
            for kc in range(KC):
                for tb in range(NB):
                    dst_fn(kc, tb, gcol0)
        else:
            for tb in range(NB):
                for kc in range(KC):
                    dst_fn(kc, tb, gcol0)

    def proj_tok(t, w_i, ncols, bnk, extra_reads=()):
        for kc in range(KC):
            k.op("pe", lambda e: e.matmul(out=ps[:, bnk, 0:ncols], lhsT=aT[:, kc, t * 128:(t + 1) * 128],
                                          rhs=wr[w_i][:, kc, 0:ncols], start=(kc == 0), stop=(kc == KC - 1)),
                 reads=[aT_r[t // 4], wr_r[w_i]], writes=[pb[bnk]], inc=(kc == KC - 1))

    def proj_fm(w_i, j, b0):
        for tb in range(NB):
            for kc in range(KC):
                k.op("pe", lambda e: e.matmul(out=ps[:, b0 + tb, :], lhsT=wr[w_i][:, kc, j * 128:(j + 1) * 128],
                                              rhs=aT[:, kc, tsl(tb)], start=(kc == 0), stop=(kc == KC - 1)),
                     reads=[aT_r[tb], wr_r[w_i]], writes=[pb[b0 + tb]], inc=(kc == KC - 1))

    def conv_fm(b0, taps, bias_c, acc, acc_r, halo=True):
        base = b0 * 512
        for tb in range(NB):
            k.op("act", lambda e: e.activation(out=acc[:, tsl(tb)], in_=ps[:, b0 + tb, :], func=AF.Identity,
                                               bias=vcol(bias_c), scale=vcol(taps[0])),
                 reads=[pb[b0 + tb], r_const], writes=[acc_r[tb]])
            for sh in range(1, len(taps)):
                lo = tb * 512
                if tb == 0:
                    k.op("dve", lambda e: e.scalar_tensor_tensor(
                        out=acc[:, sh:512], in0=ps_flat[:, base:base + 512 - sh], scalar=vcol(taps[sh]),
                        in1=acc[:, sh:512], op0=ALU.mult, op1=ALU.add),
                        reads=[pb[b0], r_const, acc_r[0]], writes=[acc_r[0]])
                else:
                    k.op("dve", lambda e: e.scalar_tensor_tensor(
                        out=acc[:, lo:lo + 512], in0=ps_flat[:, base + lo - sh:base + lo + 512 - sh],
                        scalar=vcol(taps[sh]), in1=acc[:, lo:lo + 512], op0=ALU.mult, op1=ALU.add),
                        reads=[pb[b0 + tb - 1], pb[b0 + tb], r_const, acc_r[tb]], writes=[acc_r[tb]])

    for s in range(nseq):
        dd = debug and s == 0
        arena["ptr"] = ARENA1
        sq = [A("sq%d_%d" % (s, i), [128, S], BF16) for i in range(2)]
        sq_r = [k.res("sq%d" % i) for i in range(2)]
        rstd = A("rstd%d" % s, [128, S], F32)
        rstd_r = [k.res("rstd%d" % tb) for tb in range(NB)]
        for kc in range(KC):
            k.dma("sp", out=hT[:, kc, :], in_=xT[s, kc * 128:(kc + 1) * 128, :], sem=xsem[kc], writes=hT_r[kc])

        def dst_a(kc, tb, g0):
            k.op("dve", lambda e: e.scalar_tensor_tensor(out=aT[:, kc, tsl(tb)], in0=hT[:, kc, tsl(tb)],
                                                         scalar=vcol(g0 + kc), in1=rstd[:, tsl(tb)],
                                                         op0=ALU.mult, op1=ALU.mult),
                 reads=[hT_r[kc][tb], rstd_r[tb], r_const], writes=[aT_r[tb]])

        norm_phase(hT, hT_r, V_G1, dst_a, sq, sq_r, rstd, rstd_r)
        if dd:
            dump("d_aT", aT[:, :, :], aT_r)
        if stages <= 0:
            break

        phase_end()
        arena["ptr"] = ARENA0
        QT = A("QT%d" % s, [128, 8, S], BF16)
        KT = A("KT%d" % s, [128, 4, S], BF16)
        QT_r = [k.res("QT%d" % tb) for tb in range(NB)]
        KT_r = [k.res("KT%d" % tb) for tb in range(NB)]
        Vx = A("Vx%d" % s, [128, T, 8, 65], BF16)
        Vx_r = [k.res("Vx%d" % t) for t in range(T)]
        ksum = A("ksum%d" % s, [128, 4, 8], F32)
        ksumb = A("ksumb%d" % s, [128, 4, 8], BF16)
        ksum_r = k.res("ksum")
        maskT = A("maskT%d" % s, [128, S], BF16)
        maskT_r = [k.res("maskT%d" % t) for t in range(T)]
        selb = A("selb%d" % s, [128, 64, 128], BF16)
        sel_r = k.res("sel")
        ssem = k.newsem("d_sel%d" % s)
        for q4 in range(4):
            k.dma("pool", out=selb[:, q4 * 16:(q4 + 1) * 16, :],
                  in_=sel_d[:, q4 * 2048:(q4 + 1) * 2048].rearrange("p (a b) -> p a b", b=128),
                  sem=ssem, writes=[sel_r])
        k.op("pool", lambda e: e.memset(Vx[:, :, :, 64:65], 1.0), writes=Vx_r)
        for j in range(4):
            k.op("pool", lambda e: e.memset(QT[64:128, 2 * j, :], 0.0), writes=QT_r)
            k.op("pool", lambda e: e.memset(QT[0:64, 2 * j + 1, :], 0.0), writes=QT_r)

        wq = load_w512(w_in[:, 0:512])
        wk = load_w512(w_in[:, 512:1024])
        wv = load_w512(w_in[:, 1024:1536])
        for j in range(4):
            b0 = k.half()
            proj_fm(wq, j, b0)
            for tb in range(NB):
                k.op("dve", lambda e: e.tensor_scalar(out=QT[0:64, 2 * j, tsl(tb)], in0=ps[0:64, b0 + tb, :], scalar1=0.125,
                                                      scalar2=None, op0=ALU.mult),
                     reads=[pb[b0 + tb]], writes=[QT_r[tb]])
                k.op("dve", lambda e: e.tensor_scalar(out=QT[64:128, 2 * j + 1, tsl(tb)], in0=ps[64:128, b0 + tb, :],
                                                      scalar1=0.125, scalar2=None, op0=ALU.mult),
                     reads=[pb[b0 + tb]], writes=[QT_r[tb]])
        for j in range(4):
            b0 = k.half()
            proj_fm(wk, j, b0)
            for tb in range(NB):
                for hb in range(2):
                    blk = tb * 2 + hb
                    k.op("act", lambda e: e.activation(out=KT[:, j, blk * 256:(blk + 1) * 256],
                                                       in_=ps[:, b0 + tb, hb * 256:(hb + 1) * 256], func=AF.Copy,
                                                       accum_out=ksum[:, j, blk:blk + 1]),
                         reads=[pb[b0 + tb]], writes=[KT_r[tb], ksum_r])
        k.op("dve", lambda e: e.tensor_copy(out=ksumb[:], in_=ksum[:]), reads=[ksum_r], writes=[ksum_r])
        for t in range(T):
            bnk = k.bank()
            proj_tok(t, wv, 512, bnk)
            k.op("dve", lambda e: e.tensor_copy(out=Vx[:, t, :, 0:64],
                                                in_=ps[:, bnk, :].rearrange("p (h d) -> p h d", d=64)),
                 reads=[pb[bnk]], writes=[Vx_r[t]])
        if dd:
            dump("d_QT", QT[:, :, :], QT_r)
            dump("d_KT", KT[:, :, :], KT_r)
            dump("d_Vx", Vx[:, :, :, :], Vx_r)

        if stages <= 0.5:
            break
        mb = [A("mb%d_%d" % (s, i), [128, 16, 8], F32) for i in range(2)]
        mb_r = [k.res("mb%d" % i) for i in range(2)]
        Gs = A("Gs%d" % s, [128, 8, 8], F32)
        cmp_t = A("cmp%d" % s, [128, 8, 8, 8], F32)
        rank = A("rank%d" % s, [128, 8, 8], F32)
        gs_r = k.res("gs")
        import os as _os
        for t in range(2, int(_os.environ.get("MB_MAXT", T))):
            blk = t // 2
            m = mb[t % 2]
            mr = mb_r[t % 2]
            k.op("dve", lambda e: e.memset(m[:], NEG), writes=[mr])
            if blk <= 3:
                k.op("dve", lambda e: e.memset(m[:, :, 0:blk], 0.0), writes=[mr])
            else:
                bnk = k.bank()
                for h in range(8):
                    j, hp = h // 2, (h % 2) * 64
                    k.op("pe", lambda e: e.matmul(out=ps[:, bnk, h * 8:(h + 1) * 8],
                                                  lhsT=QT[:, h, t * 128:(t + 1) * 128],
                                                  rhs=ksumb[:, j, :], start=True, stop=True),
                         reads=[QT_r[t // 4], ksum_r], writes=[pb[bnk]], inc=(h == 7))
                k.op("dve", lambda e: e.tensor_copy(out=Gs[:], in_=ps[:, bnk, 0:64].rearrange("p (h n) -> p h n", n=8)),
                     reads=[pb[bnk]], writes=[gs_r])
                if _os.environ.get("MB_NORANK"):
                    continue
                k.op("dve", lambda e: e.tensor_tensor(
                    out=cmp_t[:, :, 0:blk, 0:blk],
                    in0=Gs[:, :, 0:blk].unsqueeze(2).to_broadcast([128, 8, blk, blk]),
                    in1=Gs[:, :, 0:blk].unsqueeze(3).to_broadcast([128, 8, blk, blk]), op=ALU.is_gt),
                    reads=[gs_r], writes=[gs_r])
                k.op("dve", lambda e: e.tensor_reduce(out=rank[:, :, 0:blk], in_=cmp_t[:, :, 0:blk, 0:blk],
                                                      axis=AX.X, op=ALU.add), reads=[gs_r], writes=[gs_r])
                for c2 in range(2):
                    k.op("dve", lambda e: e.tensor_scalar(out=m[:, c2 * 8:(c2 + 1) * 8, 0:blk], in0=rank[:, :, 0:blk],
                                                          scalar1=2.5, scalar2=NEG, op0=ALU.is_gt, op1=ALU.mult),
                         reads=[gs_r], writes=[mr])
            bnk = k.bank()
            k.op("pe", lambda e: e.transpose(out=ps[:, bnk, 0:128], in_=m[:].rearrange("p a b -> p (a b)"),
                                             identity=ident_f),
                 reads=[mr, r_const], writes=[pb[bnk]])
            k.op("act", lambda e: e.activation(out=maskT[:, t * 128:(t + 1) * 128], in_=ps[:, bnk, 0:128], func=AF.Copy),
                 reads=[pb[bnk]], writes=[maskT_r[t]])
        if dd:
            dump("d_maskT", maskT[:, 256:int(_os.environ.get("MB_MAXT", T)) * 128], maskT_r[2:])

        if stages <= 0.7:
            break
        NPT = 3
        LA = 2
        PT = [A("PT%d_%d" % (s, i), [128, 512], BF16) for i in range(NPT)]
        PT_r = [k.res("PT%d" % i) for i in range(NPT)]
        attTok = [A("attTok%d_%d" % (s, i), [128, 2, 512], F32) for i in range(2)]
        attTok_r = [[k.res("attTok%d_%d" % (i, q)) for q in range(2)] for i in range(2)]
        rd = A("rd%d" % s, [128, 4], F32)
        rd_r = k.res("rd")
        sqt = [A("sqt%d_%d" % (s, i), [128, 512], F32) for i in range(2)]
        ssn = [A("ssn%d_%d" % (s, i), [128, 8], F32) for i in range(2)]
        nrm_r = [k.res("nrm%d" % i) for i in range(2)]
        attb = [A("attb%d_%d" % (s, i), [128, 512], BF16) for i in range(2)]
        attb_r = [k.res("attb%d" % i) for i in range(2)]
        units = [(i, h, n) for i in range(8) for h in range(8) for n in range(i + 1)]

        def att_A(idx):
            i, h, n = units[idx]
            j = h // 2
            q0 = i * 256
            bs = 4 + idx % 3
            p, pr = PT[idx % NPT], PT_r[idx % NPT]
            diag = (n == i)
            for half in range(2):
                kt = 2 * n + half
                qlo = 128 if (diag and half == 1) else 0
                c0 = half * 256
                k.op("pe", lambda e: e.matmul(out=ps[:, bs, c0 + qlo:c0 + 256], lhsT=KT[:, j, kt * 128:(kt + 1) * 128],
                                              rhs=QT[:, h, q0 + qlo:q0 + 256], start=True, stop=False),
                     reads=[KT_r[kt // 4], QT_r[q0 // 512]], writes=[pb[bs]], inc=False)
                if diag:
                    k.op("pe", lambda e: e.matmul(out=ps[:, bs, c0 + qlo:c0 + qlo + 128], lhsT=ident_b, rhs=tribias[:],
                                                  start=False, stop=True),
                         reads=[r_const], writes=[pb[bs]], inc=(half == 1))
                if not diag:
                    k.op("pe", lambda e: e.matmul(out=ps[:, bs, c0:c0 + 256], lhsT=selb[:, h * 8 + n, :],
                                                  rhs=maskT[:, q0:q0 + 256], start=False, stop=True),
                         reads=[sel_r, maskT_r[2 * i], maskT_r[2 * i + 1]], writes=[pb[bs]], inc=(half == 1))
            k.op("act", lambda e: e.activation(out=p[:], in_=ps[:, bs, :], func=AF.Exp),
                 reads=[pb[bs]], writes=[pr])

        def att_B(idx):
            i, h, n = units[idx]
            nkt = 2 * i + 2
            p, pr = PT[idx % NPT], PT_r[idx % NPT]
            ih = i * 8 + h
            bo = [(ih % 2) * 2, (ih % 2) * 2 + 1]
            at, at_r = attTok[i % 2], attTok_r[i % 2]
            diag = (n == i)
            for half in range(2):
                kt = 2 * n + half
                for qt in range(2):
                    if diag and half == 1 and qt == 0:
                        continue
                    first = (kt == 0)
                    last = (kt == nkt - 1) if qt == 1 else (kt == nkt - 2)
                    c0 = half * 256 + qt * 128
                    k.op("pe", lambda e: e.matmul(out=ps[:, bo[qt], 0:65], lhsT=p[:, c0:c0 + 128],
                                                  rhs=Vx[:, kt, h, :], start=first, stop=last),
                         reads=[pr, Vx_r[kt]], writes=[pb[bo[qt]]], inc=last)
            if not diag:
                return
            for qt in range(2):
                k.op("dve", lambda e: e.reciprocal(out=rd[:, qt:qt + 1], in_=ps[:, bo[qt], 64:65]),
                     reads=[pb[bo[qt]]], writes=[rd_r])
                k.op("dve", lambda e: e.tensor_scalar(out=at[:, qt, h * 64:(h + 1) * 64], in0=ps[:, bo[qt], 0:64],
                                                      scalar1=rd[:, qt:qt + 1], scalar2=None, op0=ALU.mult),
                     reads=[pb[bo[qt]], rd_r], writes=[at_r[qt]])
            if h != 7:
                return
            for qt in range(2):
                t = 2 * i + qt

                def st0(qt=qt):
                    k.op("act", lambda e: e.activation(out=sqt[qt][:], in_=at[:, qt, :], func=AF.Square),
                         reads=[at_r[qt]], writes=[nrm_r[qt]])

                def st1(qt=qt):
                    k.op("dve", lambda e: e.tensor_reduce(out=ssn[qt][:], in_=sqt[qt][:].rearrange("p (h d) -> p h d", d=64),
                                                          axis=AX.X, op=ALU.add), reads=[nrm_r[qt]], writes=[nrm_r[qt]])
                    k.op("act", lambda e: e.activation(out=ssn[qt][:], in_=ssn[qt][:], func=AF.Ln, bias=epsc[:, 0:1],
                                                       scale=1.0 / 64), reads=[nrm_r[qt], r_const], writes=[nrm_r[qt]])
                    k.op("act", lambda e: e.activation(out=ssn[qt][:], in_=ssn[qt][:], func=AF.Exp, scale=-0.5),
                         reads=[nrm_r[qt]], writes=[nrm_r[qt]])

                def st2(qt=qt):
                    k.op("dve", lambda e: e.tensor_tensor(out=attb[qt][:].rearrange("p (h d) -> p h d", d=64),
                                                          in0=at[:, qt, :].rearrange("p (h d) -> p h d", d=64),
                                                          in1=ssn[qt][:].unsqueeze(2).to_broadcast([128, 8, 64]), op=ALU.mult),
                         reads=[at_r[qt], nrm_r[qt]], writes=[attb_r[qt]])

                def st3(qt=qt, t=t):
                    bnk = 7
                    psb = ps[:, bnk, :].bitcast(BF16)
                    for jj in range(4):
                        k.op("pe", lambda e: e.transpose(out=psb[:, jj * 128:(jj + 1) * 128],
                                                         in_=attb[qt][:, jj * 128:(jj + 1) * 128], identity=ident_b),
                             reads=[attb_r[qt], r_const], writes=[pb[bnk]], inc=(jj == 3))
                    for jj in range(4):
                        k.op("dve", lambda e: e.tensor_scalar(out=mixT[:, jj, t * 128:(t + 1) * 128],
                                                              in0=psb[:, jj * 128:(jj + 1) * 128], scalar1=vcol(V_GATT + jj),
                                                              scalar2=None, op0=ALU.mult),
                             reads=[pb[bnk], r_const], writes=[mixT_r[t]])

                for kk_, f_ in enumerate((st0, st1, st2, st3)):
                    asched.setdefault(idx + 1 + 2 * kk_ + qt, []).append(f_)

        asched = {}
        for idx in range(len(units) + LA):
            if idx < len(units):
                att_A(idx)
            if idx >= LA:
                att_B(idx - LA)
                for f_ in asched.pop(idx - LA, []):
                    f_()
        for key_ in sorted(asched):
            for f_ in asched[key_]:
                f_()
        if dd:
            dump("d_mixA", mixT[:, 0:4, :], mixT_r)
        if stages <= 1:
            break
        phase_end()
        NPRE = 2
        arena["ptr"] = ARENA0 + NPRE * S * 4
        for kc in range(NPRE):
            k.dma("sp", out=hT[:, kc, :], in_=xT[s, kc * 128:(kc + 1) * 128, :], sem=xsem[kc], writes=hT_r[kc])
        mqkT = A("mqkT%d" % s, [128, 8, S], BF16)
        mqk_r = [[k.res("mqk%d_%d" % (c_, tb)) for tb in range(NB)] for c_ in range(8)]
        mkTok = A("mkTok%d" % s, [128, T, 4, 128], BF16)
        mkTok_r = [k.res("mkTok%d" % t) for t in range(T)]
        mvx = A("mvx%d" % s, [128, T, 4, 129], BF16)
        mvx_r = [k.res("mvx%d" % t) for t in range(T)]
        acc = [A("macc%d_%d" % (s, i), [128, S], F32) for i in range(1)]
        acc_r = [[k.res("macc%d_%d" % (i, tb)) for tb in range(NB)] for i in range(1)]
        gsb = A("gsb%d" % s, [128, T, 8], F32)
        ee = A("ee%d" % s, [128, T, 4], F32)
        lf = A("lf%d" % s, [128, T * 4], F32)
        CTs = A("CTs%d" % s, [128, T * 4], F32)
        rr = A("rr%d" % s, [128, T * 4], F32)
        wgt = A("wgt%d" % s, [128, T * 4], F32)
        bnd = A("bnd%d" % s, [128, T * 4], F32)
        alp = A("alp%d" % s, [128, T * 4], F32)
        alp2 = A("alp2_%d" % s, [128, T * 4], F32)
        gate_r = k.res("gate")
        k.op("pool", lambda e: e.memset(mvx[:, :, :, 128:129], 1.0), writes=mvx_r)

        wg_i = load_w512(w_in[:, 3584:3592])
        bg = k.bank()
        for t in range(T):
            for kc in range(KC):
                k.op("pe", lambda e: e.matmul(out=ps[:, bg, t * 8:(t + 1) * 8], lhsT=aT[:, kc, t * 128:(t + 1) * 128],
                                              rhs=wr[wg_i][:, kc, 0:8], start=(kc == 0), stop=(kc == KC - 1)),
                     reads=[aT_r[t // 4], wr_r[wg_i]], writes=[pb[bg]], inc=(kc == KC - 1 and t == T - 1))
        k.op("dve", lambda e: e.tensor_tensor(out=gsb[:], in0=ps[:, bg, 0:128].rearrange("p (t g) -> p t g", g=8),
                                              in1=vecs[:, V_BG:V_BG + 8].unsqueeze(1).to_broadcast([128, T, 8]), op=ALU.add),
             reads=[pb[bg], r_const], writes=[gate_r])
        k.op("act", lambda e: e.activation(out=ee[:], in_=gsb[:, :, 4:8], func=AF.Exp, scale=-1.0),
             reads=[gate_r], writes=[gate_r])
        k.op("act", lambda e: e.activation(out=ee[:], in_=ee[:], func=AF.Ln, bias=onec[:, 0:1]),
             reads=[gate_r, r_const], writes=[gate_r])
        k.op("dve", lambda e: e.tensor_scalar(out=lf[:], in0=ee[:].rearrange("p t g -> p (t g)"), scalar1=-1.0, scalar2=None,
                                              op0=ALU.mult), reads=[gate_r], writes=[gate_r])
        bb = k.bank()
        k.op("pe", lambda e: e.matmul(out=ps[:, bb, 0:64], lhsT=tri_f, rhs=lf[:], start=True, stop=True),
             reads=[gate_r, r_const], writes=[pb[bb]], inc=False)
        k.op("pe", lambda e: e.matmul(out=ps[:, bb, 64:128], lhsT=ones_f, rhs=lf[:], start=True, stop=True),
             reads=[gate_r, r_const], writes=[pb[bb]])
        k.op("dve", lambda e: e.tensor_copy(out=CTs[:], in_=ps[:, bb, 64:128]), reads=[pb[bb]], writes=[gate_r])
        k.op("dve", lambda e: e.tensor_tensor(out=rr[:], in0=CTs[:], in1=ps[:, bb, 0:64], op=ALU.subtract),
             reads=[pb[bb], gate_r], writes=[gate_r])
        k.op("dve", lambda e: e.tensor_tensor(out=wgt[:].rearrange("p (t g) -> p t g", g=4), in0=gsb[:, :, 0:4],
                                              in1=rr[:].rearrange("p (t g) -> p t g", g=4), op=ALU.add),
             reads=[gate_r], writes=[gate_r])
        k.op("act", lambda e: e.activation(out=wgt[:], in_=wgt[:], func=AF.Exp), reads=[gate_r], writes=[gate_r])
        k.op("act", lambda e: e.activation(out=bnd[:], in_=rr[:], func=AF.Exp), reads=[gate_r], writes=[gate_r])
        k.op("act", lambda e: e.activation(out=alp[:], in_=CTs[:], func=AF.Exp), reads=[gate_r], writes=[gate_r])
        k.op("dve", lambda e: e.tensor_scalar(out=alp2[:], in0=alp[:], scalar1=MD_SCALE, scalar2=None, op0=ALU.mult),
             reads=[gate_r], writes=[gate_r])

        for grp in range(2):
            w_i = load_w512(w_in[:, 1536 + grp * 512:2048 + grp * 512])
            for j in range(4):
                cj = grp * 4 + j
                b0 = k.half()
                proj_fm(w_i, j, b0)
                ai = 0
                conv_fm(b0, [V_MCW + 3 * 8 + cj, V_MCW + 2 * 8 + cj, V_MCW + 1 * 8 + cj, V_MCW + cj], V_MCB + cj,
                        acc[ai], acc_r[ai])
                for tb in range(NB):
                    k.op("act", lambda e: e.activation(out=mqkT[:, cj, tsl(tb)], in_=acc[ai][:, tsl(tb)], func=AF.Silu),
                         reads=[acc_r[ai][tb]], writes=[mqk_r[cj][tb]])
        if dd:
            dump("d_mqkT", mqkT[:, :, :], [r_ for l_ in mqk_r for r_ in l_])
        for t in range(T):
            bnk = k.bank()
            psb = ps[:, bnk, :].bitcast(BF16)
            for h in range(4):
                k.op("pe", lambda e: e.transpose(out=psb[:, h * 128:(h + 1) * 128], in_=mqkT[:, 4 + h, t * 128:(t + 1) * 128],
                                                 identity=ident_b),
                     reads=[mqk_r[4 + h][t // 4], r_const], writes=[pb[bnk]], inc=(h == 3))
            k.op("act", lambda e: e.activation(out=mkTok[:, t, :, :].rearrange("p h d -> p (h d)"), in_=psb[:, 0:512],
                                               func=AF.Copy), reads=[pb[bnk]], writes=[mkTok_r[t]])
        wv_i = load_w512(w_in[:, 2560:3072])
        for t in range(T):
            bnk = k.bank()
            proj_tok(t, wv_i, 512, bnk)
            k.op("dve", lambda e: e.tensor_copy(out=mvx[:, t, :, 0:128],
                                                in_=ps[:, bnk, :].rearrange("p (h d) -> p h d", d=128)),
                 reads=[pb[bnk]], writes=[mvx_r[t]])
        wo_i = load_w512(w_in[:, 3072:3584])
        wout_i = [load_w512(w_out[:, 0:512]), load_w512(w_out[:, 512:1024])]

        U = [A("U%d_%d" % (s, h), [128, 129], F32) for h in range(4)]
        U_r = [k.res("U%d" % h) for h in range(4)]
        Cbf = [A("Cbf%d_%d" % (s, h), [128, 129], BF16) for h in range(4)]
        Cbf_r = [k.res("Cbf%d" % h) for h in range(4)]
        NPM = 4
        PTm = [A("PTm%d_%d" % (s, i), [128, 128], BF16) for i in range(NPM)]
        PTm_r = [k.res("PTm%d" % i) for i in range(NPM)]
        mvw = [A("mvw%d_%d" % (s, i), [128, 129], BF16) for i in range(NPM)]
        mvw_r = [k.res("mvw%d" % i) for i in range(NPM)]
        nraw = [A("nraw%d_%d" % (s, i), [128, 4, 129], F32) for i in range(2)]
        nraw_r = [k.res("nraw%d" % i) for i in range(2)]
        d4 = A("d4_%d" % s, [128, 4], F32)
        rc4 = A("rc4_%d" % s, [128, 4], F32)
        d4_r = k.res("d4")
        S4 = A("S4_%d" % s, [128, 4], F32)
        s4_r = k.res("s4")
        t4 = A("t4_%d" % s, [128, 4], F32)
        t4_r = k.res("t4")
        f4 = A("f4_%d" % s, [128, 4], F32)
        f4_r = k.res("f4")
        sig = [A("sig%d_%d" % (s, i), [128, 512], F32) for i in range(3)]
        sig_r = [k.res("sig%d" % i) for i in range(3)]
        sq2 = A("sq2_%d" % s, [128, 512], F32)
        ss4 = A("ss4_%d" % s, [128, 4], F32)
        tmpn = A("tmpn%d" % s, [128, 512], F32)
        nrm2_r = k.res("nrm2")
        mixb = A("mixb%d" % s, [128, 512], BF16)
        mixb_r = k.res("mixb")
        munits = [(c, h) for c in range(T) for h in range(4)]
        mvwc = [A("mvwc%d_%d" % (s, i), [128, 4, 129], BF16) for i in range(2)]
        mvwc_r = [k.res("mvwc%d" % i) for i in range(2)]

        def mk_mvw(c):
            k.op("dve", lambda e: e.tensor_tensor(out=mvwc[c % 2][:], in0=mvx[:, c, :, :],
                                                  in1=wgt[:, c * 4:(c + 1) * 4].unsqueeze(2).to_broadcast([128, 4, 129]),
                                                  op=ALU.mult), reads=[mvx_r[c], gate_r], writes=[mvwc_r[c % 2]])

        mk_mvw(0)
        mk_mvw(1)

        def ml_A(u):
            c, h = munits[u]
            csl = slice(c * 128, (c + 1) * 128)
            col = c * 4 + h
            pm, pm_r, vw, vw_r = PTm[u % NPM], PTm_r[u % NPM], mvw[u % NPM], mvw_r[u % NPM]
            bs = u % 5
            if h == 0 and c >= 1 and c + 1 < T:
                mk_mvw(c + 1)
            vw, vw_r = mvwc[c % 2][:, h, :], mvwc_r[c % 2]
            k.op("pe", lambda e: e.matmul(out=ps[:, bs, 0:128], lhsT=mqkT[:, 4 + h, csl], rhs=mqkT[:, h, csl],
                                          start=True, stop=True),
                 reads=[mqk_r[4 + h][c // 4], mqk_r[h][c // 4]], writes=[pb[bs]])
            k.op("pe", lambda e: e.matmul(out=ps[:, bs, 128:257], lhsT=mkTok[:, c, h, :], rhs=vw, start=True, stop=True),
                 reads=[mkTok_r[c], vw_r], writes=[pb[bs]])
            k.op("dve", lambda e: e.scalar_tensor_tensor(out=pm[:], in0=ps[:, bs, 0:128], scalar=wgt[:, col:col + 1],
                                                         in1=maskU[:], op0=ALU.mult, op1=ALU.mult),
                 reads=[pb[bs], gate_r, r_const], writes=[pm_r])
            if h == 0:
                bm = 5 + c % 2
                proj_tok(c, wo_i, 512, bm)
                k.op("act", lambda e: e.activation(out=sig[c % 3][:], in_=ps[:, bm, :], func=AF.Exp, scale=-1.0),
                     reads=[pb[bm]], writes=[sig_r[c % 3]])

        def ml_B(u):
            c, h = munits[u]
            csl = slice(c * 128, (c + 1) * 128)
            col = c * 4 + h
            pm, pm_r = PTm[u % NPM], PTm_r[u % NPM]
            bs = u % 5
            bn = bs
            nr, nr_r = nraw[c % 2], nraw_r[c % 2]
            k.op("pe", lambda e: e.matmul(out=ps[:, bn, 257:386], lhsT=pm[:], rhs=mvx[:, c, h, :], start=True, stop=(c == 0)),
                 reads=[pm_r, mvx_r[c]], writes=[pb[bn]], inc=(c == 0))
            if c > 0:
                k.op("pe", lambda e: e.matmul(out=ps[:, bn, 257:386], lhsT=mqkT[:, h, csl], rhs=Cbf[h][:], start=False,
                                              stop=True),
                     reads=[mqk_r[h][c // 4], Cbf_r[h]], writes=[pb[bn]])
            k.op("dve", lambda e: e.tensor_copy(out=nr[:, h, :], in_=ps[:, bn, 257:386]),
                 reads=[pb[bn]], writes=[nr_r])
            if c == 0:
                k.op("dve", lambda e: e.tensor_copy(out=U[h][:], in_=ps[:, bs, 128:257]), reads=[pb[bs]], writes=[U_r[h]])
            else:
                k.op("dve", lambda e: e.scalar_tensor_tensor(out=U[h][:], in0=U[h][:], scalar=alp[:, col:col + 1],
                                                             in1=ps[:, bs, 128:257], op0=ALU.mult, op1=ALU.add),
                     reads=[pb[bs], gate_r, U_r[h]], writes=[U_r[h]])
            if c < T - 1:
                k.op("act", lambda e: e.activation(out=Cbf[h][:], in_=U[h][:], func=AF.Copy, scale=alp2[:, col + 4:col + 5]),
                     reads=[U_r[h], gate_r], writes=[Cbf_r[h]])
            if h != 3:
                return
            sg, sg_r = sig[c % 3], sig_r[c % 3]

            def mt0():
                k.op("dve", lambda e: e.scalar_tensor_tensor(out=d4[:], in0=nr[:, :, 128], scalar=-1.0,
                                                             in1=bnd[:, c * 4:c * 4 + 4], op0=ALU.mult, op1=ALU.max),
                     reads=[nr_r, gate_r], writes=[d4_r])
                k.op("dve", lambda e: e.tensor_tensor(out=d4[:], in0=d4[:], in1=nr[:, :, 128], op=ALU.max),
                     reads=[nr_r, d4_r], writes=[d4_r])
                k.op("dve", lambda e: e.reciprocal(out=rc4[:], in_=d4[:]), reads=[d4_r], writes=[d4_r])
                for h2 in range(4):
                    k.op("act", lambda e: e.activation(out=sq2[:, 0:128], in_=nr[:, h2, 0:128], func=AF.Square,
                                                       accum_out=S4[:, h2:h2 + 1]), reads=[nr_r], writes=[s4_r])

            def mt1():
                k.op("dve", lambda e: e.tensor_tensor(out=t4[:], in0=rc4[:], in1=rc4[:], op=ALU.mult),
                     reads=[d4_r], writes=[t4_r])
                k.op("dve", lambda e: e.tensor_tensor(out=t4[:], in0=t4[:], in1=S4[:], op=ALU.mult),
                     reads=[t4_r, s4_r], writes=[t4_r])
                k.op("act", lambda e: e.activation(out=t4[:], in_=t4[:], func=AF.Ln, bias=epsc[:, 0:1], scale=1.0 / 128),
                     reads=[t4_r, r_const], writes=[t4_r])
                k.op("act", lambda e: e.activation(out=t4[:], in_=t4[:], func=AF.Exp, scale=-0.5),
                     reads=[t4_r], writes=[t4_r])
                k.op("dve", lambda e: e.tensor_tensor(out=f4[:], in0=t4[:], in1=rc4[:], op=ALU.mult),
                     reads=[t4_r, d4_r], writes=[f4_r])
                k.op("act", lambda e: e.activation(out=sg[:], in_=sg[:], func=AF.Ln, bias=onec[:, 0:1]),
                     reads=[sg_r, r_const], writes=[sg_r])
                k.op("act", lambda e: e.activation(out=sg[:], in_=sg[:], func=AF.Exp, scale=-1.0),
                     reads=[sg_r], writes=[sg_r])

            def mt2():
                k.op("dve", lambda e: e.tensor_tensor(out=tmpn[:].rearrange("p (h d) -> p h d", d=128), in0=nr[:, :, 0:128],
                                                      in1=f4[:].unsqueeze(2).to_broadcast([128, 4, 128]), op=ALU.mult),
                     reads=[nr_r, f4_r], writes=[nrm2_r])
                k.op("pool", lambda e: e.tensor_tensor(out=mixb[:], in0=tmpn[:], in1=sg[:], op=ALU.mult),
                     reads=[nrm2_r, sg_r], writes=[mixb_r])

            def mt3():
                bt = 7
                psb = ps[:, bt, :].bitcast(BF16)
                for h2 in range(4):
                    k.op("pe", lambda e: e.transpose(out=psb[:, h2 * 128:(h2 + 1) * 128], in_=mixb[:, h2 * 128:(h2 + 1) * 128],
                                                     identity=ident_b), reads=[mixb_r, r_const], writes=[pb[bt]], inc=(h2 == 3))
                for h2 in range(4):
                    if h2 % 2 == 0:
                        k.op("act", lambda e: e.activation(out=mixT[:, 4 + h2, csl], in_=psb[:, h2 * 128:(h2 + 1) * 128],
                                                           func=AF.Copy, scale=vcol(V_GML + h2)),
                             reads=[pb[bt], r_const], writes=[mixT_r[c]])
                    else:
                        k.op("dve", lambda e: e.tensor_scalar(out=mixT[:, 4 + h2, csl], in0=psb[:, h2 * 128:(h2 + 1) * 128],
                                                              scalar1=vcol(V_GML + h2), scalar2=None, op0=ALU.mult),
                             reads=[pb[bt], r_const], writes=[mixT_r[c]])

            for kk_, f_ in enumerate((mt0, mt1, mt2, mt3)):
                msched.setdefault(u + kk_, []).append(f_)

        msched = {}
        MLA = 3
        for u in range(len(munits) + MLA):
            if u < len(munits):
                ml_A(u)
            if u >= MLA:
                ml_B(u - MLA)
                for f_ in msched.pop(u - MLA, []):
                    f_()
        for key_ in sorted(msched):
            for f_ in msched[key_]:
                f_()
        if dd:
            dump("d_mixB", mixT[:, 4:8, :], mixT_r)
        if stages <= 2:
            break
        phase_end()

        arena["ptr"] = ARENA1
        for kc in range(NPRE, KC):
            k.dma("sp", out=hT[:, kc, :], in_=xT[s, kc * 128:(kc + 1) * 128, :], sem=xsem[kc], writes=hT_r[kc])
        for dc in range(KC):
            wsl = wout_i[dc // 4]
            for tb in range(NB):
                b = k.bank()
                for kc in range(KC):
                    k.op("pe", lambda e: e.matmul(out=ps[:, b, :], lhsT=wr[wsl][:, kc, (dc % 4) * 128:(dc % 4 + 1) * 128],
                                                  rhs=mixT[:, kc, tsl(tb)], start=(kc == 0), stop=(kc == KC - 1)),
                         reads=[wr_r[wsl]] + mixT_r[tb * 4:(tb + 1) * 4], writes=[pb[b]], inc=(kc == KC - 1))
                k.op("dve", lambda e: e.tensor_tensor(out=hT[:, dc, tsl(tb)], in0=ps[:, b, :], in1=hT[:, dc, tsl(tb)],
                                                      op=ALU.add), reads=[pb[b], hT_r[dc][tb]], writes=[hT_r[dc][tb]])
        if dd:
            dump("d_hT", hT[:, :, :], [r_ for l_ in hT_r for r_ in l_])
        if stages <= 3:
            break
        phase_end()

        arena["ptr"] = ARENA1
        wd = [A("wd%d_%d" % (s, i), [128, 4, D], BF16) for i in range(2)]
        wd_r = [k.res("wd%d" % i) for i in range(2)]
        wdsem = [k.newsem("d_wd%d_%d" % (s, i)) for i in range(2)]
        ARENA_FFN = arena["ptr"]
        groups = ((0, 8), (8, 16), (16, 22))
        subs = [(gi, j0, j1, jb, min(4, j1 - jb)) for gi, (j0, j1) in enumerate(groups) for jb in range(j0, j1, 4)]
        wup = {}

        def issue_wd(gi):
            j0, j1 = groups[gi]
            for jb in range(j0, j1, 4):
                nch = min(4, j1 - jb)
                si = (jb - j0) // 4
                k.dma("pool", out=wd[si][:, 0:nch, :],
                      in_=w_down[jb * 128:(jb + nch) * 128, :].rearrange("(c p) n -> p c n", p=128),
                      sem=wdsem[si], writes=[wd_r[si]])

        def issue_g(sb):
            _, _, _, jb, nch = subs[sb]
            wup[(sb, 0)] = load_w512(w_up[:, jb * 128:(jb + nch) * 128])

        def issue_v(sb):
            _, _, _, jb, nch = subs[sb]
            wup[(sb, 1)] = load_w512(w_up[:, (NFC + jb) * 128:(NFC + jb + nch) * 128])

        issue_g(0)
        issue_v(0)
        issue_wd(0)
        sqn = [A("sqn%d_%d" % (s, i), [128, S], BF16) for i in range(2)]
        sqn_r = [k.res("sqn%d" % i) for i in range(2)]
        rstd2 = A("rstd2_%d" % s, [128, S], F32)
        rstd2_r = [k.res("rstd2_%d" % tb) for tb in range(NB)]

        def dst_a2(kc, tb, g0):
            k.op("dve", lambda e: e.scalar_tensor_tensor(out=aT[:, kc, tsl(tb)], in0=hT[:, kc, tsl(tb)],
                                                         scalar=vcol(g0 + kc), in1=rstd2[:, tsl(tb)],
                                                         op0=ALU.mult, op1=ALU.mult),
                 reads=[hT_r[kc][tb], rstd2_r[tb], r_const], writes=[aT_r[tb]])

        norm_phase(hT, hT_r, V_G2, dst_a2, sqn, sqn_r, rstd2, rstd2_r)
        phase_end()

        arena["ptr"] = ARENA_FFN
        G = A("G%d" % s, [128, 8, S], BF16, at=MIXT_OFF)
        G_r = [[k.res("G%d_%d" % (jj, tb)) for tb in range(NB)] for jj in range(8)]
        accg = A("accg%d" % s, [128, S], F32)
        accg_r = [k.res("accg%d" % tb) for tb in range(NB)]
        accv = A("accv%d" % s, [128, S], F32)
        accv_r = [k.res("accv%d" % tb) for tb in range(NB)]
        sgt = A("sgt%d" % s, [128, S], F32)
        sgt_r = [k.res("sgt%d" % tb) for tb in range(NB)]
        for sb, (gi, j0, j1, jb, nch) in enumerate(subs):
            if sb + 1 < len(subs):
                issue_g(sb + 1)
            wg_i, wv_i = wup[(sb, 0)], wup[(sb, 1)]
            for jj in range(nch):
                j = jb + jj
                jg = j - j0
                b0 = k.half()
                proj_fm(wg_i, jj, b0)
                conv_fm(b0, [V_FCW + 2 * 44 + j, V_FCW + 44 + j, V_FCW + j], V_FCB + j, accg, accg_r)
                for tb in range(NB):
                    k.op("act", lambda e: e.activation(out=sgt[:, tsl(tb)], in_=accg[:, tsl(tb)], func=AF.Silu),
                         reads=[accg_r[tb]], writes=[sgt_r[tb]])
                b1 = k.half()
                proj_fm(wv_i, jj, b1)
                jv = NFC + j
                conv_fm(b1, [V_FCW + 2 * 44 + jv, V_FCW + 44 + jv, V_FCW + jv], V_FCB + jv, accv, accv_r)
                for tb in range(NB):
                    k.op("pool", lambda e: e.tensor_tensor(out=G[:, jg, tsl(tb)], in0=sgt[:, tsl(tb)],
                                                           in1=accv[:, tsl(tb)], op=ALU.mult),
                         reads=[sgt_r[tb], accv_r[tb]], writes=[G_r[jg][tb]])
            if sb + 1 < len(subs):
                issue_v(sb + 1)
            if jb + nch < j1:
                continue
            ng = j1 - j0
            for dc in range(KC):
                for tb in range(NB):
                    b = k.bank()
                    for jg in range(ng):
                        k.op("pe", lambda e: e.matmul(out=ps[:, b, :], lhsT=wd[jg // 4][:, jg % 4, dc * 128:(dc + 1) * 128],
                                                      rhs=G[:, jg, tsl(tb)], start=(jg == 0), stop=(jg == ng - 1)),
                             reads=[wd_r[jg // 4], G_r[jg][tb]], writes=[pb[b]], inc=(jg == ng - 1))
                    k.op("dve", lambda e: e.tensor_tensor(out=hT[:, dc, tsl(tb)], in0=ps[:, b, :], in1=hT[:, dc, tsl(tb)],
                                                          op=ALU.add), reads=[pb[b], hT_r[dc][tb]], writes=[hT_r[dc][tb]])
            if gi + 1 < len(groups):
                issue_wd(gi + 1)
        if stages <= 4:
            break
        phase_end()

        arena["ptr"] = ARENA1
        sqf = [A("sqf%d_%d" % (s, i), [128, S], BF16) for i in range(2)]
        sqf_r = [k.res("sqf%d" % i) for i in range(2)]
        rstd3 = A("rstd3_%d" % s, [128, S], F32)
        rstd3_r = [k.res("rstd3_%d" % tb) for tb in range(NB)]

        def dst_o(kc, tb, g0):
            k.op("dve", lambda e: e.scalar_tensor_tensor(out=hT[:, kc, tsl(tb)], in0=hT[:, kc, tsl(tb)],
                                                         scalar=vcol(g0 + kc), in1=rstd3[:, tsl(tb)],
                                                         op0=ALU.mult, op1=ALU.mult),
                 reads=[hT_r[kc][tb], rstd3_r[tb], r_const], writes=[hT_r[kc][tb]])
            k.dma("sp", out=outT[s, kc * 128:(kc + 1) * 128, tsl(tb)], in_=hT[:, kc, tsl(tb)], sem=osem[kc],
                  reads=[hT_r[kc][tb]])

        norm_phase(hT, hT_r, V_GF, dst_o, sqf, sqf_r, rstd3, rstd3_r, kc_outer=True)
        phase_end()
    for o_ in osem + [gsem]:
        if o_.n:
            k.engs["sp"].wait_ge(o_.h, o_.n)
    return nc, dbg, k


def host_prep(inputs):
    f = np.float32
    x = np.asarray(inputs["x"], f)
    vecs = np.zeros((128, NVEC), f)

    def cols(v):
        v = np.asarray(v, f).reshape(-1)
        return v.reshape(-1, 128).T

    vecs[:, V_G1:V_G1 + 8] = cols(inputs["norm_mix_g"][0])
    vecs[:, V_G2:V_G2 + 8] = cols(inputs["norm_ffn_g"][0])
    vecs[:, V_GF:V_GF + 8] = cols(inputs["norm_final_g"])
    vecs[:, V_GATT:V_GATT + 4] = cols(inputs["att_out_g"][0])
    vecs[:, V_GML:V_GML + 4] = cols(inputs["mlstm_out_g"][0])
    mcw = np.asarray(inputs["mlstm_conv_w"][0], f)
    for j in range(4):
        vecs[:, V_MCW + j * 8:V_MCW + (j + 1) * 8] = cols(mcw[j])
    vecs[:, V_MCB:V_MCB + 8] = cols(inputs["mlstm_conv_b"][0])
    fcw = np.asarray(inputs["ffn_conv_w"][0], f)
    for j in range(3):
        vecs[:, V_FCW + j * 44:V_FCW + (j + 1) * 44] = cols(fcw[j])
    vecs[:, V_FCB:V_FCB + 44] = cols(inputs["ffn_conv_b"][0])
    vecs[:, V_BG:V_BG + 8] = np.asarray(inputs["b_gates"][0], f)[None, :]
    cstf = np.zeros((128, NCST), f)
    cstf[:, C_ID:C_ID + 128] = np.eye(128, dtype=f)
    cstf[:, C_TRI:C_TRI + 128] = np.triu(np.ones((128, 128), f))
    cstf[:, C_ONE:C_ONE + 128] = 1.0
    sel = np.zeros((128, 64, 128), f)
    for p in range(64):
        sel[p, p, :] = 1.0
    common = {
        "w_in": np.ascontiguousarray(inputs["w_in"][0], f),
        "w_out": np.ascontiguousarray(inputs["w_out"][0], f),
        "w_up": np.ascontiguousarray(inputs["w_up"][0], f),
        "w_down": np.ascontiguousarray(inputs["w_down"][0], f),
        "vecs": vecs, "cstf": cstf, "sel": sel.reshape(128, 64 * 128),
    }
    in_maps = []
    for c in range(8):
        m = dict(common)
        m["xT"] = np.ascontiguousarray(np.transpose(x[2 * c:2 * c + 2], (0, 2, 1)))
        in_maps.append(m)
    return in_maps


def kernel(**inputs):
    nc, _, _ = build()
    in_maps = host_prep(inputs)
    res = run_bass_kernel_spmd(nc, in_maps, core_ids=list(range(8)))
    out = np.empty((16, S, D), np.float32)
    for c in range(8):
        o = np.asarray(res.results[c]["outT"])
        out[2 * c:2 * c + 2] = np.transpose(o, (0, 2, 1))
    return out
```

```python
import numpy as np
import concourse.bass as bass
import concourse.mybir as mybir
from concourse.bass_utils import run_bass_kernel_spmd

F32 = mybir.dt.float32
BF16 = mybir.dt.bfloat16
ALU = mybir.AluOpType
AF = mybir.ActivationFunctionType
AX = mybir.AxisListType

S = 2048
D = 1024
KC = 8
T = 16
NB = 4
DFF = 2816
NFC = 22
PROJ = 3592
NEG = -30000.0
EPS = 1e-6
MD_SCALE = 128.0 ** -0.5

V_G1, V_G2, V_GF, V_GATT, V_GML, V_MCW, V_MCB, V_FCW, V_FCB, V_BG, NVEC = 0, 8, 16, 24, 28, 32, 64, 72, 204, 248, 256
C_ID, C_TRI, C_ONE, NCST = 0, 128, 256, 384


class Res:
    __slots__ = ("name", "w", "r")

    def __init__(self, name):
        self.name = name
        self.w = None
        self.r = {}


class Sem:
    __slots__ = ("h", "n")

    def __init__(self, h):
        self.h = h
        self.n = 0


class K:
    def __init__(self, nc):
        self.nc = nc
        self.engs = {"pe": nc.tensor, "act": nc.scalar, "dve": nc.vector, "pool": nc.gpsimd, "sp": nc.sync}
        self.esem = {e: Sem(nc.alloc_semaphore("e_" + e)) for e in ("pe", "act", "dve", "pool")}
        self.seen = {e: {} for e in self.engs}
        self.nwait = 0
        self.bank_i = 0
        self.half_i = 0

    def res(self, name):
        return Res(name)

    def newsem(self, name):
        return Sem(self.nc.alloc_semaphore(name))

    def _deps(self, eng, reads, writes):
        deps = []
        for r in reads:
            if r.w is not None:
                deps.append(r.w)
        for w in writes:
            if w.w is not None and w.w[2] != eng:
                deps.append(w.w)
            for d in w.r.values():
                if d[2] != eng:
                    deps.append(d)
        return deps

    def _wait(self, eng, deps):
        best = {}
        for sem, val, src in deps:
            if eng == "pe" and src == "pe":
                continue
            key = id(sem)
            if key not in best or best[key][1] < val:
                best[key] = (sem, val)
        seen = self.seen[eng]
        for key, (sem, val) in best.items():
            if seen.get(key, 0) < val:
                self.engs[eng].wait_ge(sem.h, val)
                seen[key] = val
                self.nwait += 1

    def _record(self, tok, reads, writes):
        for r in reads:
            key = id(tok[0])
            if key not in r.r or r.r[key][1] < tok[1]:
                r.r[key] = tok
        for w in writes:
            w.w = tok
            w.r = {}

    def op(self, eng, fn, reads=(), writes=(), inc=True):
        self._wait(eng, self._deps(eng, reads, writes))
        ins = fn(self.engs[eng])
        s = self.esem[eng]
        if inc:
            s.n += 1
            ins.then_inc(s.h, 1)
            tok = (s, s.n, eng)
        else:
            tok = (s, s.n + 1, eng)
        self._record(tok, reads, writes)
        return ins

    def dma(self, q, out, in_, sem, reads=(), writes=(), **kw):
        qn = "dma_" + q
        self._wait(q, self._deps(qn, reads, writes))
        ins = self.engs[q].dma_start(out=out, in_=in_, **kw)
        sem.n += 16
        ins.then_inc(sem.h, 16)
        self._record((sem, sem.n, qn), reads, writes)
        return ins

    def bank(self):
        b = self.bank_i
        self.bank_i = (self.bank_i + 1) % 8
        return b

    def half(self):
        b = self.half_i * 4
        self.half_i ^= 1
        self.bank_i = (b + 4) % 8
        return b

    def barrier(self):
        self.nc.all_engine_barrier()


def build(debug=False, nseq=2, stages=99):
    nc = bass.Bass("TRN2", target_bir_lowering=False)
    k = K(nc)
    dbg = {}

    xT = nc.dram_tensor("xT", [2, D, S], F32, kind="ExternalInput").ap()
    w_in = nc.dram_tensor("w_in", [D, PROJ], F32, kind="ExternalInput").ap()
    w_out = nc.dram_tensor("w_out", [D, D], F32, kind="ExternalInput").ap()
    w_up = nc.dram_tensor("w_up", [D, 2 * DFF], F32, kind="ExternalInput").ap()
    w_down = nc.dram_tensor("w_down", [DFF, D], F32, kind="ExternalInput").ap()
    vecs_d = nc.dram_tensor("vecs", [128, NVEC], F32, kind="ExternalInput").ap()
    cstf_d = nc.dram_tensor("cstf", [128, NCST], F32, kind="ExternalInput").ap()
    sel_d = nc.dram_tensor("sel", [128, 64 * 128], F32, kind="ExternalInput").ap()
    outT = nc.dram_tensor("outT", [2, D, S], F32, kind="ExternalOutput").ap()

    DTB = {F32: 4, BF16: 2}
    arena = {"ptr": ((nc.sbuf_base + 63) // 64) * 64, "top": nc.sbuf_top}

    def A(name, shape, dt, at=None):
        nb = DTB[dt]
        for d_ in shape[1:]:
            nb *= d_
        off = arena["ptr"] if at is None else at
        if at is None:
            arena["ptr"] = ((off + nb + 63) // 64) * 64
            assert arena["ptr"] <= arena["top"], "SBUF arena overflow at %s: %d > %d" % (name, arena["ptr"], arena["top"])
        return nc.alloc_sbuf_tensor_at("s_" + name, shape, dt, offset=off)

    ps = nc.alloc_psum_tensor("ps", [128, 8, 512], F32)
    pb = [k.res("pb%d" % i) for i in range(8)]
    ps_flat = ps[:, :, :].rearrange("p a b -> p (a b)")

    vecs = A("vecs", [128, NVEC], F32)
    cstf = A("cstf", [128, NCST], F32)
    cstb = A("cstb", [128, NCST], BF16)
    maskU = A("maskU", [128, 128], F32)
    tribias = A("tribias", [128, 128], BF16)
    epsc = A("epsc", [128, 1], F32)
    onec = A("onec", [128, 1], F32)
    r_const = k.res("const")
    dsem = k.newsem("d_const")
    osem = [k.newsem("d_out%d" % kc) for kc in range(KC)]
    gsem = k.newsem("d_dbg")
    xsem = [k.newsem("d_x%d" % kc) for kc in range(KC)]

    hT_r = [[k.res("hT%d_%d" % (kc, tb)) for tb in range(NB)] for kc in range(KC)]
    aT = A("aT", [128, KC, S], BF16)
    aT_r = [k.res("aT%d" % tb) for tb in range(NB)]
    MIXT_OFF = arena["ptr"]
    mixT = A("mixT", [128, KC, S], BF16)
    mixT_r = [k.res("mixT%d" % t) for t in range(T)]
    NW = 3
    wr = [A("wr%d" % i, [128, KC, 512], BF16) for i in range(NW)]
    wr_r = [k.res("wr%d" % i) for i in range(NW)]
    wsem = [k.newsem("d_w%d" % i) for i in range(NW)]
    wri = [0]

    def wslot():
        i = wri[0]
        wri[0] = (i + 1) % NW
        return i

    def load_w512(src_cols_ap):
        i = wslot()
        n = src_cols_ap.shape[1]
        k.dma("pool", out=wr[i][:, :, 0:n], in_=src_cols_ap.rearrange("(kc p) n -> p kc n", p=128),
              sem=wsem[i], writes=[wr_r[i]])
        return i

    ARENA0 = arena["ptr"]
    hT = A("hT", [128, KC, S], F32)
    ARENA1 = arena["ptr"]

    def phase_end():
        if debug and gsem.n:
            k.engs["sp"].wait_ge(gsem.h, gsem.n)
        k.barrier()

    k.dma("sp", out=vecs[:], in_=vecs_d, sem=dsem, writes=[r_const])
    k.dma("sp", out=cstf[:], in_=cstf_d, sem=dsem, writes=[r_const])
    k.op("dve", lambda e: e.tensor_copy(out=cstb[:], in_=cstf[:]), reads=[r_const], writes=[r_const])
    k.op("dve", lambda e: e.tensor_scalar(out=maskU[:], in0=cstf[:, C_TRI:C_TRI + 128], scalar1=MD_SCALE,
                                          scalar2=None, op0=ALU.mult), reads=[r_const], writes=[r_const])
    k.op("dve", lambda e: e.tensor_scalar(out=tribias[:], in0=cstf[:, C_TRI:C_TRI + 128], scalar1=-1.0,
                                          scalar2=-NEG, op0=ALU.add, op1=ALU.mult), reads=[r_const], writes=[r_const])
    k.op("dve", lambda e: e.memset(epsc[:], EPS), writes=[r_const])
    k.op("dve", lambda e: e.memset(onec[:], 1.0), writes=[r_const])
    ident_b = cstb[:, C_ID:C_ID + 128]
    ident_f = cstf[:, C_ID:C_ID + 128]
    tri_b = cstb[:, C_TRI:C_TRI + 128]
    tri_f = cstf[:, C_TRI:C_TRI + 128]
    ones_b = cstb[:, C_ONE:C_ONE + 128]
    ones_f = cstf[:, C_ONE:C_ONE + 128]

    def vcol(c):
        return vecs[:, c:c + 1]

    def dump(name, ap, reads):
        if not debug:
            return
        t = nc.dram_tensor(name, list(ap.shape), ap.dtype, kind="ExternalOutput").ap()
        k.dma("sp", out=t, in_=ap, sem=gsem, reads=reads)
        dbg[name] = t

    def tsl(tb):
        return slice(tb * 512, (tb + 1) * 512)

    def norm_phase(src, src_r, gcol0, dst_fn, sq, sq_r, rstd, rstd_r, kc_outer=False):
        b0 = k.half()
        for kc in range(KC):
            i = kc % 2
            k.op("act", lambda e: e.activation(out=sq[i][:], in_=src[:, kc, :], func=AF.Square),
                 reads=src_r[kc], writes=[sq_r[i]])
            for tb in range(NB):
                k.op("pe", lambda e: e.matmul(out=ps[:, b0 + tb, :], lhsT=ones_b, rhs=sq[i][:, tsl(tb)],
                                              start=(kc == 0), stop=(kc == KC - 1)),
                     reads=[sq_r[i], r_const], writes=[pb[b0 + tb]], inc=(tb == NB - 1))
        for tb in range(NB):
            k.op("act", lambda e: e.activation(out=rstd[:, tsl(tb)], in_=ps[:, b0 + tb, :], func=AF.Ln,
                                               bias=epsc[:, 0:1], scale=1.0 / D),
                 reads=[pb[b0 + tb], r_const], writes=[rstd_r[tb]])
            k.op("act", lambda e: e.activation(out=rstd[:, tsl(tb)], in_=rstd[:, tsl(tb)], func=AF.Exp, scale=-0.5),
                 reads=[rstd_r[tb]], writes=[rstd_r[tb]])
        if kc_outer:
            for kc in range(KC):
                for tb in range(NB):
                    dst_fn(kc, tb, gcol0)
        else:
            for tb in range(NB):
                for kc in range(KC):
                    dst_fn(kc, tb, gcol0)

    def proj_tok(t, w_i, ncols, bnk, extra_reads=()):
        for kc in range(KC):
            k.op("pe", lambda e: e.matmul(out=ps[:, bnk, 0:ncols], lhsT=aT[:, kc, t * 128:(t + 1) * 128],
                                          rhs=wr[w_i][:, kc, 0:ncols], start=(kc == 0), stop=(kc == KC - 1)),
                 reads=[aT_r[t // 4], wr_r[w_i]], writes=[pb[bnk]], inc=(kc == KC - 1))

    def proj_fm(w_i, j, b0):
        for tb in range(NB):
            for kc in range(KC):
                k.op("pe", lambda e: e.matmul(out=ps[:, b0 + tb, :], lhsT=wr[w_i][:, kc, j * 128:(j + 1) * 128],
                                              rhs=aT[:, kc, tsl(tb)], start=(kc == 0), stop=(kc == KC - 1)),
                     reads=[aT_r[tb], wr_r[w_i]], writes=[pb[b0 + tb]], inc=(kc == KC - 1))

    def conv_fm(b0, taps, bias_c, acc, acc_r, halo=True):
        base = b0 * 512
        for tb in range(NB):
            k.op("act", lambda e: e.activation(out=acc[:, tsl(tb)], in_=ps[:, b0 + tb, :], func=AF.Identity,
                                               bias=vcol(bias_c), scale=vcol(taps[0])),
                 reads=[pb[b0 + tb], r_const], writes=[acc_r[tb]])
            for sh in range(1, len(taps)):
                lo = tb * 512
                if tb == 0:
                    k.op("dve", lambda e: e.scalar_tensor_tensor(
                        out=acc[:, sh:512], in0=ps_flat[:, base:base + 512 - sh], scalar=vcol(taps[sh]),
                        in1=acc[:, sh:512], op0=ALU.mult, op1=ALU.add),
                        reads=[pb[b0], r_const, acc_r[0]], writes=[acc_r[0]])
                else:
                    k.op("dve", lambda e: e.scalar_tensor_tensor(
                        out=acc[:, lo:lo + 512], in0=ps_flat[:, base + lo - sh:base + lo + 512 - sh],
                        scalar=vcol(taps[sh]), in1=acc[:, lo:lo + 512], op0=ALU.mult, op1=ALU.add),
                        reads=[pb[b0 + tb - 1], pb[b0 + tb], r_const, acc_r[tb]], writes=[acc_r[tb]])

    p1a_pre = []
    for s in range(nseq):
        dd = debug and s == 0
        arena["ptr"] = ARENA1
        sq = [A("sq%d_%d" % (s, i), [128, S], BF16) for i in range(2)]
        sq_r = [k.res("sq%d" % i) for i in range(2)]
        rstd = A("rstd%d" % s, [128, S], F32)
        rstd_r = [k.res("rstd%d" % tb) for tb in range(NB)]
        for kc in range(KC):
            k.dma("sp", out=hT[:, kc, :], in_=xT[s, kc * 128:(kc + 1) * 128, :], sem=xsem[kc], writes=hT_r[kc])

        def dst_a(kc, tb, g0):
            k.op("dve", lambda e: e.scalar_tensor_tensor(out=aT[:, kc, tsl(tb)], in0=hT[:, kc, tsl(tb)],
                                                         scalar=vcol(g0 + kc), in1=rstd[:, tsl(tb)],
                                                         op0=ALU.mult, op1=ALU.mult),
                 reads=[hT_r[kc][tb], rstd_r[tb], r_const], writes=[aT_r[tb]])

        norm_phase(hT, hT_r, V_G1, dst_a, sq, sq_r, rstd, rstd_r)
        if dd:
            dump("d_aT", aT[:, :, :], aT_r)
        if stages <= 0:
            break

        phase_end()
        arena["ptr"] = ARENA0
        QT = A("QT%d" % s, [128, 8, S], BF16)
        KT = A("KT%d" % s, [128, 4, S], BF16)
        QT_r = [k.res("QT%d" % tb) for tb in range(NB)]
        KT_r = [k.res("KT%d" % tb) for tb in range(NB)]
        Vx = A("Vx%d" % s, [128, T, 8, 65], BF16)
        Vx_r = [k.res("Vx%d" % t) for t in range(T)]
        ksum = A("ksum%d" % s, [128, 4, 8], F32)
        ksumb = A("ksumb%d" % s, [128, 4, 8], BF16)
        ksum_r = k.res("ksum")
        maskT = A("maskT%d" % s, [128, S], BF16)
        maskT_r = [k.res("maskT%d" % t) for t in range(T)]
        selb = A("selb%d" % s, [128, 64, 128], BF16)
        sel_r = k.res("sel")
        ssem = k.newsem("d_sel%d" % s)
        if p1a_pre:
            wq, wk, wv = p1a_pre.pop()
        else:
            wq = load_w512(w_in[:, 0:512])
            wk = load_w512(w_in[:, 512:1024])
            wv = load_w512(w_in[:, 1024:1536])
        k.op("pool", lambda e: e.memset(Vx[:, :, :, 64:65], 1.0), writes=Vx_r)
        for j in range(4):
            k.op("pool", lambda e: e.memset(QT[64:128, 2 * j, :], 0.0), writes=QT_r)
            k.op("pool", lambda e: e.memset(QT[0:64, 2 * j + 1, :], 0.0), writes=QT_r)

        for q4 in range(4):
            k.dma("pool", out=selb[:, q4 * 16:(q4 + 1) * 16, :],
                  in_=sel_d[:, q4 * 2048:(q4 + 1) * 2048].rearrange("p (a b) -> p a b", b=128),
                  sem=ssem, writes=[sel_r])
        for j in range(4):
            b0 = k.half()
            proj_fm(wq, j, b0)
            for tb in range(NB):
                k.op("dve", lambda e: e.tensor_scalar(out=QT[0:64, 2 * j, tsl(tb)], in0=ps[0:64, b0 + tb, :], scalar1=0.125,
                                                      scalar2=None, op0=ALU.mult),
                     reads=[pb[b0 + tb]], writes=[QT_r[tb]])
                k.op("dve", lambda e: e.tensor_scalar(out=QT[64:128, 2 * j + 1, tsl(tb)], in0=ps[64:128, b0 + tb, :],
                                                      scalar1=0.125, scalar2=None, op0=ALU.mult),
                     reads=[pb[b0 + tb]], writes=[QT_r[tb]])
        for j in range(4):
            b0 = k.half()
            proj_fm(wk, j, b0)
            for tb in range(NB):
                for hb in range(2):
                    blk = tb * 2 + hb
                    k.op("act", lambda e: e.activation(out=KT[:, j, blk * 256:(blk + 1) * 256],
                                                       in_=ps[:, b0 + tb, hb * 256:(hb + 1) * 256], func=AF.Copy,
                                                       accum_out=ksum[:, j, blk:blk + 1]),
                         reads=[pb[b0 + tb]], writes=[KT_r[tb], ksum_r])
        k.op("dve", lambda e: e.tensor_copy(out=ksumb[:], in_=ksum[:]), reads=[ksum_r], writes=[ksum_r])
        mb = [A("mb%d_%d" % (s, i), [128, 16, 8], F32) for i in range(2)]
        mb_r = [k.res("mb%d" % i) for i in range(2)]
        Gs = A("Gs%d" % s, [128, 8, 8], F32)
        cmp_t = A("cmp%d" % s, [128, 8, 8, 8], F32)
        rank = A("rank%d" % s, [128, 8, 8], F32)
        gs_r = k.res("gs")

        def vproj(t):
            bnk = k.bank()
            proj_tok(t, wv, 512, bnk)
            k.op("dve", lambda e: e.tensor_copy(out=Vx[:, t, :, 0:64],
                                                in_=ps[:, bnk, :].rearrange("p (h d) -> p h d", d=64)),
                 reads=[pb[bnk]], writes=[Vx_r[t]])

        def mask_front(t):
            blk = t // 2
            m = mb[t % 2]
            mr = mb_r[t % 2]
            k.op("dve", lambda e: e.memset(m[:], NEG), writes=[mr])
            if blk <= 3:
                k.op("dve", lambda e: e.memset(m[:, :, 0:blk], 0.0), writes=[mr])
                return
            bnk = k.bank()
            for h in range(8):
                j = h // 2
                k.op("pe", lambda e: e.matmul(out=ps[:, bnk, h * 8:(h + 1) * 8],
                                              lhsT=QT[:, h, t * 128:(t + 1) * 128],
                                              rhs=ksumb[:, j, :], start=True, stop=True),
                     reads=[QT_r[t // 4], ksum_r], writes=[pb[bnk]], inc=(h == 7))
            k.op("dve", lambda e: e.tensor_copy(out=Gs[:], in_=ps[:, bnk, 0:64].rearrange("p (h n) -> p h n", n=8)),
                 reads=[pb[bnk]], writes=[gs_r])
            k.op("dve", lambda e: e.tensor_tensor(
                out=cmp_t[:, :, 0:blk, 0:blk],
                in0=Gs[:, :, 0:blk].unsqueeze(2).to_broadcast([128, 8, blk, blk]),
                in1=Gs[:, :, 0:blk].unsqueeze(3).to_broadcast([128, 8, blk, blk]), op=ALU.is_gt),
                reads=[gs_r], writes=[gs_r])
            k.op("dve", lambda e: e.tensor_reduce(out=rank[:, :, 0:blk], in_=cmp_t[:, :, 0:blk, 0:blk],
                                                  axis=AX.X, op=ALU.add), reads=[gs_r], writes=[gs_r])
            for c2 in range(2):
                k.op("dve", lambda e: e.tensor_scalar(out=m[:, c2 * 8:(c2 + 1) * 8, 0:blk], in0=rank[:, :, 0:blk],
                                                      scalar1=2.5, scalar2=NEG, op0=ALU.is_gt, op1=ALU.mult),
                     reads=[gs_r], writes=[mr])

        def mask_back(t):
            m = mb[t % 2]
            mr = mb_r[t % 2]
            bnk = k.bank()
            k.op("pe", lambda e: e.transpose(out=ps[:, bnk, 0:128], in_=m[:].rearrange("p a b -> p (a b)"),
                                             identity=ident_f),
                 reads=[mr, r_const], writes=[pb[bnk]])
            k.op("act", lambda e: e.activation(out=maskT[:, t * 128:(t + 1) * 128], in_=ps[:, bnk, 0:128], func=AF.Copy),
                 reads=[pb[bnk]], writes=[maskT_r[t]])

        for t in range(T + 1):
            if 2 <= t < T:
                mask_front(t)
            if t < T:
                vproj(t)
            if 2 <= t - 1 < T:
                mask_back(t - 1)
        if dd:
            dump("d_QT", QT[:, :, :], QT_r)
            dump("d_KT", KT[:, :, :], KT_r)
            dump("d_Vx", Vx[:, :, :, :], Vx_r)
        if dd:
            dump("d_maskT", maskT[:, 256:], maskT_r[2:])

        if stages <= 0.7:
            break
        p1b_pre = [load_w512(w_in[:, 3584:3592]), load_w512(w_in[:, 1536:2048]), load_w512(w_in[:, 2048:2560])]
        NPT = 3
        LA = 2
        PT = [A("PT%d_%d" % (s, i), [128, 512], BF16) for i in range(NPT)]
        PT_r = [k.res("PT%d" % i) for i in range(NPT)]
        attTok = [A("attTok%d_%d" % (s, i), [128, 2, 512], F32) for i in range(2)]
        attTok_r = [[k.res("attTok%d_%d" % (i, q)) for q in range(2)] for i in range(2)]
        rd = A("rd%d" % s, [128, 4], F32)
        rd_r = k.res("rd")
        sqt = [A("sqt%d_%d" % (s, i), [128, 512], F32) for i in range(2)]
        ssn = [A("ssn%d_%d" % (s, i), [128, 8], F32) for i in range(2)]
        nrm_r = [k.res("nrm%d" % i) for i in range(2)]
        attb = [A("attb%d_%d" % (s, i), [128, 512], BF16) for i in range(2)]
        attb_r = [k.res("attb%d" % i) for i in range(2)]
        units = [(i, h, n) for i in range(8) for h in range(8) for n in range(i + 1)]

        def att_A(idx):
            i, h, n = units[idx]
            j = h // 2
            q0 = i * 256
            bs = 4 + idx % 3
            p, pr = PT[idx % NPT], PT_r[idx % NPT]
            diag = (n == i)
            for half in range(2):
                kt = 2 * n + half
                qlo = 128 if (diag and half == 1) else 0
                c0 = half * 256
                k.op("pe", lambda e: e.matmul(out=ps[:, bs, c0 + qlo:c0 + 256], lhsT=KT[:, j, kt * 128:(kt + 1) * 128],
                                              rhs=QT[:, h, q0 + qlo:q0 + 256], start=True, stop=False),
                     reads=[KT_r[kt // 4], QT_r[q0 // 512]], writes=[pb[bs]], inc=False)
                if diag:
                    k.op("pe", lambda e: e.matmul(out=ps[:, bs, c0 + qlo:c0 + qlo + 128], lhsT=ident_b, rhs=tribias[:],
                                                  start=False, stop=True),
                         reads=[r_const], writes=[pb[bs]], inc=(half == 1))
                if not diag:
                    k.op("pe", lambda e: e.matmul(out=ps[:, bs, c0:c0 + 256], lhsT=selb[:, h * 8 + n, :],
                                                  rhs=maskT[:, q0:q0 + 256], start=False, stop=True),
                         reads=[sel_r, maskT_r[2 * i], maskT_r[2 * i + 1]], writes=[pb[bs]], inc=(half == 1))
            k.op("act", lambda e: e.activation(out=p[:], in_=ps[:, bs, :], func=AF.Exp),
                 reads=[pb[bs]], writes=[pr])

        def att_B(idx):
            i, h, n = units[idx]
            nkt = 2 * i + 2
            p, pr = PT[idx % NPT], PT_r[idx % NPT]
            ih = i * 8 + h
            bo = [(ih % 2) * 2, (ih % 2) * 2 + 1]
            at, at_r = attTok[i % 2], attTok_r[i % 2]
            diag = (n == i)
            for half in range(2):
                kt = 2 * n + half
                for qt in range(2):
                    if diag and half == 1 and qt == 0:
                        continue
                    first = (kt == 0)
                    last = (kt == nkt - 1) if qt == 1 else (kt == nkt - 2)
                    c0 = half * 256 + qt * 128
                    k.op("pe", lambda e: e.matmul(out=ps[:, bo[qt], 0:65], lhsT=p[:, c0:c0 + 128],
                                                  rhs=Vx[:, kt, h, :], start=first, stop=last),
                         reads=[pr, Vx_r[kt]], writes=[pb[bo[qt]]], inc=last)
            if not diag:
                return
            for qt in range(2):
                k.op("dve", lambda e: e.reciprocal(out=rd[:, qt:qt + 1], in_=ps[:, bo[qt], 64:65]),
                     reads=[pb[bo[qt]]], writes=[rd_r])
                k.op("dve", lambda e: e.tensor_scalar(out=at[:, qt, h * 64:(h + 1) * 64], in0=ps[:, bo[qt], 0:64],
                                                      scalar1=rd[:, qt:qt + 1], scalar2=None, op0=ALU.mult),
                     reads=[pb[bo[qt]], rd_r], writes=[at_r[qt]])
            if h != 7:
                return
            for qt in range(2):
                t = 2 * i + qt

                def st0(qt=qt):
                    k.op("act", lambda e: e.activation(out=sqt[qt][:], in_=at[:, qt, :], func=AF.Square),
                         reads=[at_r[qt]], writes=[nrm_r[qt]])

                def st1(qt=qt):
                    k.op("dve", lambda e: e.tensor_reduce(out=ssn[qt][:], in_=sqt[qt][:].rearrange("p (h d) -> p h d", d=64),
                                                          axis=AX.X, op=ALU.add), reads=[nrm_r[qt]], writes=[nrm_r[qt]])
                    k.op("act", lambda e: e.activation(out=ssn[qt][:], in_=ssn[qt][:], func=AF.Ln, bias=epsc[:, 0:1],
                                                       scale=1.0 / 64), reads=[nrm_r[qt], r_const], writes=[nrm_r[qt]])
                    k.op("act", lambda e: e.activation(out=ssn[qt][:], in_=ssn[qt][:], func=AF.Exp, scale=-0.5),
                         reads=[nrm_r[qt]], writes=[nrm_r[qt]])

                def st2(qt=qt):
                    k.op("dve", lambda e: e.tensor_tensor(out=attb[qt][:].rearrange("p (h d) -> p h d", d=64),
                                                          in0=at[:, qt, :].rearrange("p (h d) -> p h d", d=64),
                                                          in1=ssn[qt][:].unsqueeze(2).to_broadcast([128, 8, 64]), op=ALU.mult),
                         reads=[at_r[qt], nrm_r[qt]], writes=[attb_r[qt]])

                def st3(qt=qt, t=t):
                    bnk = 7
                    psb = ps[:, bnk, :].bitcast(BF16)
                    for jj in range(4):
                        k.op("pe", lambda e: e.transpose(out=psb[:, jj * 128:(jj + 1) * 128],
                                                         in_=attb[qt][:, jj * 128:(jj + 1) * 128], identity=ident_b),
                             reads=[attb_r[qt], r_const], writes=[pb[bnk]], inc=(jj == 3))
                    for jj in range(4):
                        k.op("dve", lambda e: e.tensor_scalar(out=mixT[:, jj, t * 128:(t + 1) * 128],
                                                              in0=psb[:, jj * 128:(jj + 1) * 128], scalar1=vcol(V_GATT + jj),
                                                              scalar2=None, op0=ALU.mult),
                             reads=[pb[bnk], r_const], writes=[mixT_r[t]])

                for kk_, f_ in enumerate((st0, st1, st2, st3)):
                    asched.setdefault(idx + 1 + 2 * kk_ + qt, []).append(f_)

        asched = {}
        for idx in range(len(units) + LA):
            if idx < len(units):
                att_A(idx)
            if idx >= LA:
                att_B(idx - LA)
                for f_ in asched.pop(idx - LA, []):
                    f_()
        for key_ in sorted(asched):
            for f_ in asched[key_]:
                f_()
        if dd:
            dump("d_mixA", mixT[:, 0:4, :], mixT_r)
        if stages <= 1:
            break
        phase_end()
        NPRE = 2
        arena["ptr"] = ARENA0 + NPRE * S * 4
        for kc in range(NPRE):
            k.dma("sp", out=hT[:, kc, :], in_=xT[s, kc * 128:(kc + 1) * 128, :], sem=xsem[kc], writes=hT_r[kc])
        mqkT = A("mqkT%d" % s, [128, 8, S], BF16)
        mqk_r = [[k.res("mqk%d_%d" % (c_, tb)) for tb in range(NB)] for c_ in range(8)]
        mkTok = A("mkTok%d" % s, [128, T, 4, 128], BF16)
        mkTok_r = [k.res("mkTok%d" % t) for t in range(T)]
        mvx = A("mvx%d" % s, [128, T, 4, 129], BF16)
        mvx_r = [k.res("mvx%d" % t) for t in range(T)]
        acc = [A("macc%d_%d" % (s, i), [128, S], F32) for i in range(1)]
        acc_r = [[k.res("macc%d_%d" % (i, tb)) for tb in range(NB)] for i in range(1)]
        gsb = A("gsb%d" % s, [128, T, 8], F32)
        ee = A("ee%d" % s, [128, T, 4], F32)
        lf = A("lf%d" % s, [128, T * 4], F32)
        CTs = A("CTs%d" % s, [128, T * 4], F32)
        rr = A("rr%d" % s, [128, T * 4], F32)
        wgt = A("wgt%d" % s, [128, T * 4], F32)
        bnd = A("bnd%d" % s, [128, T * 4], F32)
        alp = A("alp%d" % s, [128, T * 4], F32)
        alp2 = A("alp2_%d" % s, [128, T * 4], F32)
        gate_r = k.res("gate")
        k.op("pool", lambda e: e.memset(mvx[:, :, :, 128:129], 1.0), writes=mvx_r)

        wg_i = p1b_pre[0]
        bg = k.bank()
        for t in range(T):
            for kc in range(KC):
                k.op("pe", lambda e: e.matmul(out=ps[:, bg, t * 8:(t + 1) * 8], lhsT=aT[:, kc, t * 128:(t + 1) * 128],
                                              rhs=wr[wg_i][:, kc, 0:8], start=(kc == 0), stop=(kc == KC - 1)),
                     reads=[aT_r[t // 4], wr_r[wg_i]], writes=[pb[bg]], inc=(kc == KC - 1 and t == T - 1))
        k.op("dve", lambda e: e.tensor_tensor(out=gsb[:], in0=ps[:, bg, 0:128].rearrange("p (t g) -> p t g", g=8),
                                              in1=vecs[:, V_BG:V_BG + 8].unsqueeze(1).to_broadcast([128, T, 8]), op=ALU.add),
             reads=[pb[bg], r_const], writes=[gate_r])
        k.op("act", lambda e: e.activation(out=ee[:], in_=gsb[:, :, 4:8], func=AF.Exp, scale=-1.0),
             reads=[gate_r], writes=[gate_r])
        k.op("act", lambda e: e.activation(out=ee[:], in_=ee[:], func=AF.Ln, bias=onec[:, 0:1]),
             reads=[gate_r, r_const], writes=[gate_r])
        k.op("dve", lambda e: e.tensor_scalar(out=lf[:], in0=ee[:].rearrange("p t g -> p (t g)"), scalar1=-1.0, scalar2=None,
                                              op0=ALU.mult), reads=[gate_r], writes=[gate_r])
        bb = k.bank()
        k.op("pe", lambda e: e.matmul(out=ps[:, bb, 0:64], lhsT=tri_f, rhs=lf[:], start=True, stop=True),
             reads=[gate_r, r_const], writes=[pb[bb]], inc=False)
        k.op("pe", lambda e: e.matmul(out=ps[:, bb, 64:128], lhsT=ones_f, rhs=lf[:], start=True, stop=True),
             reads=[gate_r, r_const], writes=[pb[bb]])
        k.op("dve", lambda e: e.tensor_copy(out=CTs[:], in_=ps[:, bb, 64:128]), reads=[pb[bb]], writes=[gate_r])
        k.op("dve", lambda e: e.tensor_tensor(out=rr[:], in0=CTs[:], in1=ps[:, bb, 0:64], op=ALU.subtract),
             reads=[pb[bb], gate_r], writes=[gate_r])
        k.op("dve", lambda e: e.tensor_tensor(out=wgt[:].rearrange("p (t g) -> p t g", g=4), in0=gsb[:, :, 0:4],
                                              in1=rr[:].rearrange("p (t g) -> p t g", g=4), op=ALU.add),
             reads=[gate_r], writes=[gate_r])
        k.op("act", lambda e: e.activation(out=wgt[:], in_=wgt[:], func=AF.Exp), reads=[gate_r], writes=[gate_r])
        k.op("act", lambda e: e.activation(out=bnd[:], in_=rr[:], func=AF.Exp), reads=[gate_r], writes=[gate_r])
        k.op("act", lambda e: e.activation(out=alp[:], in_=CTs[:], func=AF.Exp), reads=[gate_r], writes=[gate_r])
        k.op("dve", lambda e: e.tensor_scalar(out=alp2[:], in0=alp[:], scalar1=MD_SCALE, scalar2=None, op0=ALU.mult),
             reads=[gate_r], writes=[gate_r])

        for grp in range(2):
            w_i = p1b_pre[1 + grp]
            for j in range(4):
                cj = grp * 4 + j
                b0 = k.half()
                proj_fm(w_i, j, b0)
                ai = 0
                conv_fm(b0, [V_MCW + 3 * 8 + cj, V_MCW + 2 * 8 + cj, V_MCW + 1 * 8 + cj, V_MCW + cj], V_MCB + cj,
                        acc[ai], acc_r[ai])
                for tb in range(NB):
                    k.op("act", lambda e: e.activation(out=mqkT[:, cj, tsl(tb)], in_=acc[ai][:, tsl(tb)], func=AF.Silu),
                         reads=[acc_r[ai][tb]], writes=[mqk_r[cj][tb]])
        if dd:
            dump("d_mqkT", mqkT[:, :, :], [r_ for l_ in mqk_r for r_ in l_])
        for t in range(T):
            bnk = k.bank()
            psb = ps[:, bnk, :].bitcast(BF16)
            for h in range(4):
                k.op("pe", lambda e: e.transpose(out=psb[:, h * 128:(h + 1) * 128], in_=mqkT[:, 4 + h, t * 128:(t + 1) * 128],
                                                 identity=ident_b),
                     reads=[mqk_r[4 + h][t // 4], r_const], writes=[pb[bnk]], inc=(h == 3))
            k.op("act", lambda e: e.activation(out=mkTok[:, t, :, :].rearrange("p h d -> p (h d)"), in_=psb[:, 0:512],
                                               func=AF.Copy), reads=[pb[bnk]], writes=[mkTok_r[t]])
        wv_i = load_w512(w_in[:, 2560:3072])
        for t in range(T):
            bnk = k.bank()
            proj_tok(t, wv_i, 512, bnk)
            k.op("dve", lambda e: e.tensor_copy(out=mvx[:, t, :, 0:128],
                                                in_=ps[:, bnk, :].rearrange("p (h d) -> p h d", d=128)),
                 reads=[pb[bnk]], writes=[mvx_r[t]])
        wo_i = load_w512(w_in[:, 3072:3584])
        wout_i = [load_w512(w_out[:, 0:512]), load_w512(w_out[:, 512:1024])]

        U = [A("U%d_%d" % (s, h), [128, 129], F32) for h in range(4)]
        U_r = [k.res("U%d" % h) for h in range(4)]
        Cbf = [A("Cbf%d_%d" % (s, h), [128, 129], BF16) for h in range(4)]
        Cbf_r = [k.res("Cbf%d" % h) for h in range(4)]
        NPM = 4
        PTm = [A("PTm%d_%d" % (s, i), [128, 128], BF16) for i in range(NPM)]
        PTm_r = [k.res("PTm%d" % i) for i in range(NPM)]
        mvw = [A("mvw%d_%d" % (s, i), [128, 129], BF16) for i in range(NPM)]
        mvw_r = [k.res("mvw%d" % i) for i in range(NPM)]
        nraw = [A("nraw%d_%d" % (s, i), [128, 4, 129], F32) for i in range(2)]
        nraw_r = [k.res("nraw%d" % i) for i in range(2)]
        d4 = A("d4_%d" % s, [128, 4], F32)
        rc4 = A("rc4_%d" % s, [128, 4], F32)
        d4_r = k.res("d4")
        S4 = A("S4_%d" % s, [128, 4], F32)
        s4_r = k.res("s4")
        t4 = A("t4_%d" % s, [128, 4], F32)
        t4_r = k.res("t4")
        f4 = A("f4_%d" % s, [128, 4], F32)
        f4_r = k.res("f4")
        sig = [A("sig%d_%d" % (s, i), [128, 512], F32) for i in range(3)]
        sig_r = [k.res("sig%d" % i) for i in range(3)]
        sq2 = A("sq2_%d" % s, [128, 512], F32)
        ss4 = A("ss4_%d" % s, [128, 4], F32)
        tmpn = A("tmpn%d" % s, [128, 512], F32)
        nrm2_r = k.res("nrm2")
        mixb = A("mixb%d" % s, [128, 512], BF16)
        mixb_r = k.res("mixb")
        munits = [(c, h) for c in range(T) for h in range(4)]
        mvwc = [A("mvwc%d_%d" % (s, i), [128, 4, 129], BF16) for i in range(2)]
        mvwc_r = [k.res("mvwc%d" % i) for i in range(2)]

        def mk_mvw(c):
            k.op("dve", lambda e: e.tensor_tensor(out=mvwc[c % 2][:], in0=mvx[:, c, :, :],
                                                  in1=wgt[:, c * 4:(c + 1) * 4].unsqueeze(2).to_broadcast([128, 4, 129]),
                                                  op=ALU.mult), reads=[mvx_r[c], gate_r], writes=[mvwc_r[c % 2]])

        mk_mvw(0)
        mk_mvw(1)

        def ml_A(u):
            c, h = munits[u]
            csl = slice(c * 128, (c + 1) * 128)
            col = c * 4 + h
            pm, pm_r, vw, vw_r = PTm[u % NPM], PTm_r[u % NPM], mvw[u % NPM], mvw_r[u % NPM]
            bs = u % 5
            if h == 0 and c >= 1 and c + 1 < T:
                mk_mvw(c + 1)
            vw, vw_r = mvwc[c % 2][:, h, :], mvwc_r[c % 2]
            k.op("pe", lambda e: e.matmul(out=ps[:, bs, 0:128], lhsT=mqkT[:, 4 + h, csl], rhs=mqkT[:, h, csl],
                                          start=True, stop=True),
                 reads=[mqk_r[4 + h][c // 4], mqk_r[h][c // 4]], writes=[pb[bs]])
            k.op("pe", lambda e: e.matmul(out=ps[:, bs, 128:257], lhsT=mkTok[:, c, h, :], rhs=vw, start=True, stop=True),
                 reads=[mkTok_r[c], vw_r], writes=[pb[bs]])
            k.op("dve", lambda e: e.scalar_tensor_tensor(out=pm[:], in0=ps[:, bs, 0:128], scalar=wgt[:, col:col + 1],
                                                         in1=maskU[:], op0=ALU.mult, op1=ALU.mult),
                 reads=[pb[bs], gate_r, r_const], writes=[pm_r])
            if h == 0:
                bm = 5 + c % 2
                proj_tok(c, wo_i, 512, bm)
                k.op("act", lambda e: e.activation(out=sig[c % 3][:], in_=ps[:, bm, :], func=AF.Exp, scale=-1.0),
                     reads=[pb[bm]], writes=[sig_r[c % 3]])

        def ml_B(u):
            c, h = munits[u]
            csl = slice(c * 128, (c + 1) * 128)
            col = c * 4 + h
            pm, pm_r = PTm[u % NPM], PTm_r[u % NPM]
            bs = u % 5
            bn = bs
            nr, nr_r = nraw[c % 2], nraw_r[c % 2]
            k.op("pe", lambda e: e.matmul(out=ps[:, bn, 257:386], lhsT=pm[:], rhs=mvx[:, c, h, :], start=True, stop=(c == 0)),
                 reads=[pm_r, mvx_r[c]], writes=[pb[bn]], inc=(c == 0))
            if c > 0:
                k.op("pe", lambda e: e.matmul(out=ps[:, bn, 257:386], lhsT=mqkT[:, h, csl], rhs=Cbf[h][:], start=False,
                                              stop=True),
                     reads=[mqk_r[h][c // 4], Cbf_r[h]], writes=[pb[bn]])
            k.op("dve", lambda e: e.tensor_copy(out=nr[:, h, :], in_=ps[:, bn, 257:386]),
                 reads=[pb[bn]], writes=[nr_r])
            if c == 0:
                k.op("dve", lambda e: e.tensor_copy(out=U[h][:], in_=ps[:, bs, 128:257]), reads=[pb[bs]], writes=[U_r[h]])
            else:
                k.op("dve", lambda e: e.scalar_tensor_tensor(out=U[h][:], in0=U[h][:], scalar=alp[:, col:col + 1],
                                                             in1=ps[:, bs, 128:257], op0=ALU.mult, op1=ALU.add),
                     reads=[pb[bs], gate_r, U_r[h]], writes=[U_r[h]])
            if c < T - 1:
                k.op("act", lambda e: e.activation(out=Cbf[h][:], in_=U[h][:], func=AF.Copy, scale=alp2[:, col + 4:col + 5]),
                     reads=[U_r[h], gate_r], writes=[Cbf_r[h]])
            if h != 3:
                return
            sg, sg_r = sig[c % 3], sig_r[c % 3]

            def mt0():
                k.op("dve", lambda e: e.scalar_tensor_tensor(out=d4[:], in0=nr[:, :, 128], scalar=-1.0,
                                                             in1=bnd[:, c * 4:c * 4 + 4], op0=ALU.mult, op1=ALU.max),
                     reads=[nr_r, gate_r], writes=[d4_r])
                k.op("dve", lambda e: e.tensor_tensor(out=d4[:], in0=d4[:], in1=nr[:, :, 128], op=ALU.max),
                     reads=[nr_r, d4_r], writes=[d4_r])
                k.op("dve", lambda e: e.reciprocal(out=rc4[:], in_=d4[:]), reads=[d4_r], writes=[d4_r])
                for h2 in range(4):
                    k.op("act", lambda e: e.activation(out=sq2[:, 0:128], in_=nr[:, h2, 0:128], func=AF.Square,
                                                       accum_out=S4[:, h2:h2 + 1]), reads=[nr_r], writes=[s4_r])

            def mt1():
                k.op("dve", lambda e: e.tensor_tensor(out=t4[:], in0=rc4[:], in1=rc4[:], op=ALU.mult),
                     reads=[d4_r], writes=[t4_r])
                k.op("dve", lambda e: e.tensor_tensor(out=t4[:], in0=t4[:], in1=S4[:], op=ALU.mult),
                     reads=[t4_r, s4_r], writes=[t4_r])
                k.op("act", lambda e: e.activation(out=t4[:], in_=t4[:], func=AF.Ln, bias=epsc[:, 0:1], scale=1.0 / 128),
                     reads=[t4_r, r_const], writes=[t4_r])
                k.op("act", lambda e: e.activation(out=t4[:], in_=t4[:], func=AF.Exp, scale=-0.5),
                     reads=[t4_r], writes=[t4_r])
                k.op("dve", lambda e: e.tensor_tensor(out=f4[:], in0=t4[:], in1=rc4[:], op=ALU.mult),
                     reads=[t4_r, d4_r], writes=[f4_r])
                k.op("act", lambda e: e.activation(out=sg[:], in_=sg[:], func=AF.Ln, bias=onec[:, 0:1]),
                     reads=[sg_r, r_const], writes=[sg_r])
                k.op("act", lambda e: e.activation(out=sg[:], in_=sg[:], func=AF.Exp, scale=-1.0),
                     reads=[sg_r], writes=[sg_r])

            def mt2():
                k.op("dve", lambda e: e.tensor_tensor(out=tmpn[:].rearrange("p (h d) -> p h d", d=128), in0=nr[:, :, 0:128],
                                                      in1=f4[:].unsqueeze(2).to_broadcast([128, 4, 128]), op=ALU.mult),
                     reads=[nr_r, f4_r], writes=[nrm2_r])
                k.op("pool", lambda e: e.tensor_tensor(out=mixb[:], in0=tmpn[:], in1=sg[:], op=ALU.mult),
                     reads=[nrm2_r, sg_r], writes=[mixb_r])

            def mt3():
                bt = 7
                psb = ps[:, bt, :].bitcast(BF16)
                for h2 in range(4):
                    k.op("pe", lambda e: e.transpose(out=psb[:, h2 * 128:(h2 + 1) * 128], in_=mixb[:, h2 * 128:(h2 + 1) * 128],
                                                     identity=ident_b), reads=[mixb_r, r_const], writes=[pb[bt]], inc=(h2 == 3))
                for h2 in range(4):
                    if h2 % 2 == 0:
                        k.op("act", lambda e: e.activation(out=mixT[:, 4 + h2, csl], in_=psb[:, h2 * 128:(h2 + 1) * 128],
                                                           func=AF.Copy, scale=vcol(V_GML + h2)),
                             reads=[pb[bt], r_const], writes=[mixT_r[c]])
                    else:
                        k.op("dve", lambda e: e.tensor_scalar(out=mixT[:, 4 + h2, csl], in0=psb[:, h2 * 128:(h2 + 1) * 128],
                                                              scalar1=vcol(V_GML + h2), scalar2=None, op0=ALU.mult),
                             reads=[pb[bt], r_const], writes=[mixT_r[c]])

            for kk_, f_ in enumerate((mt0, mt1, mt2, mt3)):
                msched.setdefault(u + kk_, []).append(f_)

        msched = {}
        MLA = 3
        for u in range(len(munits) + MLA):
            if u < len(munits):
                ml_A(u)
            if u >= MLA:
                ml_B(u - MLA)
                for f_ in msched.pop(u - MLA, []):
                    f_()
        for key_ in sorted(msched):
            for f_ in msched[key_]:
                f_()
        if dd:
            dump("d_mixB", mixT[:, 4:8, :], mixT_r)
        if stages <= 2:
            break
        phase_end()

        arena["ptr"] = ARENA1
        for kc in range(NPRE, KC):
            k.dma("sp", out=hT[:, kc, :], in_=xT[s, kc * 128:(kc + 1) * 128, :], sem=xsem[kc], writes=hT_r[kc])
        for dc in range(KC):
            wsl = wout_i[dc // 4]
            for tb in range(NB):
                b = k.bank()
                for kc in range(KC):
                    k.op("pe", lambda e: e.matmul(out=ps[:, b, :], lhsT=wr[wsl][:, kc, (dc % 4) * 128:(dc % 4 + 1) * 128],
                                                  rhs=mixT[:, kc, tsl(tb)], start=(kc == 0), stop=(kc == KC - 1)),
                         reads=[wr_r[wsl]] + mixT_r[tb * 4:(tb + 1) * 4], writes=[pb[b]], inc=(kc == KC - 1))
                k.op("dve", lambda e: e.tensor_tensor(out=hT[:, dc, tsl(tb)], in0=ps[:, b, :], in1=hT[:, dc, tsl(tb)],
                                                      op=ALU.add), reads=[pb[b], hT_r[dc][tb]], writes=[hT_r[dc][tb]])
        if dd:
            dump("d_hT", hT[:, :, :], [r_ for l_ in hT_r for r_ in l_])
        if stages <= 3:
            break
        phase_end()

        arena["ptr"] = ARENA1
        wd = [A("wd%d_%d" % (s, i), [128, 4, D], BF16) for i in range(2)]
        wd_r = [k.res("wd%d" % i) for i in range(2)]
        wdsem = [k.newsem("d_wd%d_%d" % (s, i)) for i in range(2)]
        ARENA_FFN = arena["ptr"]
        groups = ((0, 8), (8, 16), (16, 22))
        subs = [(gi, j0, j1, jb, min(4, j1 - jb)) for gi, (j0, j1) in enumerate(groups) for jb in range(j0, j1, 4)]
        wup = {}

        def issue_wd(gi):
            j0, j1 = groups[gi]
            for jb in range(j0, j1, 4):
                nch = min(4, j1 - jb)
                si = (jb - j0) // 4
                k.dma("pool", out=wd[si][:, 0:nch, :],
                      in_=w_down[jb * 128:(jb + nch) * 128, :].rearrange("(c p) n -> p c n", p=128),
                      sem=wdsem[si], writes=[wd_r[si]])

        def issue_g(sb):
            _, _, _, jb, nch = subs[sb]
            wup[(sb, 0)] = load_w512(w_up[:, jb * 128:(jb + nch) * 128])

        def issue_v(sb):
            _, _, _, jb, nch = subs[sb]
            wup[(sb, 1)] = load_w512(w_up[:, (NFC + jb) * 128:(NFC + jb + nch) * 128])

        issue_g(0)
        issue_v(0)
        issue_wd(0)
        sqn = [A("sqn%d_%d" % (s, i), [128, S], BF16) for i in range(2)]
        sqn_r = [k.res("sqn%d" % i) for i in range(2)]
        rstd2 = A("rstd2_%d" % s, [128, S], F32)
        rstd2_r = [k.res("rstd2_%d" % tb) for tb in range(NB)]

        def dst_a2(kc, tb, g0):
            k.op("dve", lambda e: e.scalar_tensor_tensor(out=aT[:, kc, tsl(tb)], in0=hT[:, kc, tsl(tb)],
                                                         scalar=vcol(g0 + kc), in1=rstd2[:, tsl(tb)],
                                                         op0=ALU.mult, op1=ALU.mult),
                 reads=[hT_r[kc][tb], rstd2_r[tb], r_const], writes=[aT_r[tb]])

        norm_phase(hT, hT_r, V_G2, dst_a2, sqn, sqn_r, rstd2, rstd2_r)
        phase_end()

        arena["ptr"] = ARENA_FFN
        G = A("G%d" % s, [128, 8, S], BF16, at=MIXT_OFF)
        G_r = [[k.res("G%d_%d" % (jj, tb)) for tb in range(NB)] for jj in range(8)]
        accg = A("accg%d" % s, [128, S], F32)
        accg_r = [k.res("accg%d" % tb) for tb in range(NB)]
        accv = A("accv%d" % s, [128, S], F32)
        accv_r = [k.res("accv%d" % tb) for tb in range(NB)]
        sgt = A("sgt%d" % s, [128, S], F32)
        sgt_r = [k.res("sgt%d" % tb) for tb in range(NB)]
        for sb, (gi, j0, j1, jb, nch) in enumerate(subs):
            if sb + 1 < len(subs):
                issue_g(sb + 1)
            wg_i, wv_i = wup[(sb, 0)], wup[(sb, 1)]
            for jj in range(nch):
                j = jb + jj
                jg = j - j0
                b0 = k.half()
                proj_fm(wg_i, jj, b0)
                conv_fm(b0, [V_FCW + 2 * 44 + j, V_FCW + 44 + j, V_FCW + j], V_FCB + j, accg, accg_r)
                for tb in range(NB):
                    k.op("act", lambda e: e.activation(out=sgt[:, tsl(tb)], in_=accg[:, tsl(tb)], func=AF.Silu),
                         reads=[accg_r[tb]], writes=[sgt_r[tb]])
                b1 = k.half()
                proj_fm(wv_i, jj, b1)
                jv = NFC + j
                conv_fm(b1, [V_FCW + 2 * 44 + jv, V_FCW + 44 + jv, V_FCW + jv], V_FCB + jv, accv, accv_r)
                for tb in range(NB):
                    k.op("pool", lambda e: e.tensor_tensor(out=G[:, jg, tsl(tb)], in0=sgt[:, tsl(tb)],
                                                           in1=accv[:, tsl(tb)], op=ALU.mult),
                         reads=[sgt_r[tb], accv_r[tb]], writes=[G_r[jg][tb]])
            if sb + 1 < len(subs):
                issue_v(sb + 1)
            if jb + nch < j1:
                continue
            ng = j1 - j0
            for dc in range(KC):
                for tb in range(NB):
                    b = k.bank()
                    for jg in range(ng):
                        k.op("pe", lambda e: e.matmul(out=ps[:, b, :], lhsT=wd[jg // 4][:, jg % 4, dc * 128:(dc + 1) * 128],
                                                      rhs=G[:, jg, tsl(tb)], start=(jg == 0), stop=(jg == ng - 1)),
                             reads=[wd_r[jg // 4], G_r[jg][tb]], writes=[pb[b]], inc=(jg == ng - 1))
                    k.op("dve", lambda e: e.tensor_tensor(out=hT[:, dc, tsl(tb)], in0=ps[:, b, :], in1=hT[:, dc, tsl(tb)],
                                                          op=ALU.add), reads=[pb[b], hT_r[dc][tb]], writes=[hT_r[dc][tb]])
            if gi + 1 < len(groups):
                issue_wd(gi + 1)
        if stages <= 4:
            break
        phase_end()

        arena["ptr"] = ARENA1
        if s + 1 < nseq:
            p1a_pre.append((load_w512(w_in[:, 0:512]), load_w512(w_in[:, 512:1024]), load_w512(w_in[:, 1024:1536])))
        sqf = [A("sqf%d_%d" % (s, i), [128, S], BF16) for i in range(2)]
        sqf_r = [k.res("sqf%d" % i) for i in range(2)]
        rstd3 = A("rstd3_%d" % s, [128, S], F32)
        rstd3_r = [k.res("rstd3_%d" % tb) for tb in range(NB)]

        def dst_o(kc, tb, g0):
            k.op("dve", lambda e: e.scalar_tensor_tensor(out=hT[:, kc, tsl(tb)], in0=hT[:, kc, tsl(tb)],
                                                         scalar=vcol(g0 + kc), in1=rstd3[:, tsl(tb)],
                                                         op0=ALU.mult, op1=ALU.mult),
                 reads=[hT_r[kc][tb], rstd3_r[tb], r_const], writes=[hT_r[kc][tb]])
            k.dma("sp", out=outT[s, kc * 128:(kc + 1) * 128, tsl(tb)], in_=hT[:, kc, tsl(tb)], sem=osem[kc],
                  reads=[hT_r[kc][tb]])

        norm_phase(hT, hT_r, V_GF, dst_o, sqf, sqf_r, rstd3, rstd3_r, kc_outer=True)
        phase_end()
    for o_ in osem + [gsem]:
        if o_.n:
            k.engs["sp"].wait_ge(o_.h, o_.n)
    return nc, dbg, k


def host_prep(inputs):
    f = np.float32
    x = np.asarray(inputs["x"], f)
    vecs = np.zeros((128, NVEC), f)

    def cols(v):
        v = np.asarray(v, f).reshape(-1)
        return v.reshape(-1, 128).T

    vecs[:, V_G1:V_G1 + 8] = cols(inputs["norm_mix_g"][0])
    vecs[:, V_G2:V_G2 + 8] = cols(inputs["norm_ffn_g"][0])
    vecs[:, V_GF:V_GF + 8] = cols(inputs["norm_final_g"])
    vecs[:, V_GATT:V_GATT + 4] = cols(inputs["att_out_g"][0])
    vecs[:, V_GML:V_GML + 4] = cols(inputs["mlstm_out_g"][0])
    mcw = np.asarray(inputs["mlstm_conv_w"][0], f)
    for j in range(4):
        vecs[:, V_MCW + j * 8:V_MCW + (j + 1) * 8] = cols(mcw[j])
    vecs[:, V_MCB:V_MCB + 8] = cols(inputs["mlstm_conv_b"][0])
    fcw = np.asarray(inputs["ffn_conv_w"][0], f)
    for j in range(3):
        vecs[:, V_FCW + j * 44:V_FCW + (j + 1) * 44] = cols(fcw[j])
    vecs[:, V_FCB:V_FCB + 44] = cols(inputs["ffn_conv_b"][0])
    vecs[:, V_BG:V_BG + 8] = np.asarray(inputs["b_gates"][0], f)[None, :]
    cstf = np.zeros((128, NCST), f)
    cstf[:, C_ID:C_ID + 128] = np.eye(128, dtype=f)
    cstf[:, C_TRI:C_TRI + 128] = np.triu(np.ones((128, 128), f))
    cstf[:, C_ONE:C_ONE + 128] = 1.0
    sel = np.zeros((128, 64, 128), f)
    for p in range(64):
        sel[p, p, :] = 1.0
    common = {
        "w_in": np.ascontiguousarray(inputs["w_in"][0], f),
        "w_out": np.ascontiguousarray(inputs["w_out"][0], f),
        "w_up": np.ascontiguousarray(inputs["w_up"][0], f),
        "w_down": np.ascontiguousarray(inputs["w_down"][0], f),
        "vecs": vecs, "cstf": cstf, "sel": sel.reshape(128, 64 * 128),
    }
    in_maps = []
    for c in range(8):
        m = dict(common)
        m["xT"] = np.ascontiguousarray(np.transpose(x[2 * c:2 * c + 2], (0, 2, 1)))
        in_maps.append(m)
    return in_maps


def kernel(**inputs):
    nc, _, _ = build()
    in_maps = host_prep(inputs)
    res = run_bass_kernel_spmd(nc, in_maps, core_ids=list(range(8)))
    out = np.empty((16, S, D), np.float32)
    for c in range(8):
        o = np.asarray(res.results[c]["outT"])
        out[2 * c:2 * c + 2] = np.transpose(o, (0, 2, 1))
    return out
```

```python
import numpy as np
import concourse.bass as bass
import concourse.mybir as mybir
from concourse.bass_utils import run_bass_kernel_spmd

F32 = mybir.dt.float32
BF16 = mybir.dt.bfloat16
ALU = mybir.AluOpType
AF = mybir.ActivationFunctionType
AX = mybir.AxisListType

S = 2048
D = 1024
KC = 8
T = 16
NB = 4
DFF = 2816
NFC = 22
PROJ = 3592
NEG = -30000.0
EPS = 1e-6
MD_SCALE = 128.0 ** -0.5

V_G1, V_G2, V_GF, V_GATT, V_GML, V_MCW, V_MCB, V_FCW, V_FCB, V_BG, NVEC = 0, 8, 16, 24, 28, 32, 64, 72, 204, 248, 256
C_ID, C_TRI, C_ONE, NCST = 0, 128, 256, 384


class Res:
    __slots__ = ("name", "w", "r")

    def __init__(self, name):
        self.name = name
        self.w = None
        self.r = {}


class Sem:
    __slots__ = ("h", "n")

    def __init__(self, h):
        self.h = h
        self.n = 0


class K:
    def __init__(self, nc):
        self.nc = nc
        self.engs = {"pe": nc.tensor, "act": nc.scalar, "dve": nc.vector, "pool": nc.gpsimd, "sp": nc.sync}
        self.esem = {e: Sem(nc.alloc_semaphore("e_" + e)) for e in ("pe", "act", "dve", "pool")}
        self.seen = {e: {} for e in self.engs}
        self.nwait = 0
        self.bank_i = 0
        self.half_i = 0

    def res(self, name):
        return Res(name)

    def newsem(self, name):
        return Sem(self.nc.alloc_semaphore(name))

    def _deps(self, eng, reads, writes):
        deps = []
        for r in reads:
            if r.w is not None:
                deps.append(r.w)
        for w in writes:
            if w.w is not None and w.w[2] != eng:
                deps.append(w.w)
            for d in w.r.values():
                if d[2] != eng:
                    deps.append(d)
        return deps

    def _wait(self, eng, deps):
        best = {}
        for sem, val, src in deps:
            if eng == "pe" and src == "pe":
                continue
            key = id(sem)
            if key not in best or best[key][1] < val:
                best[key] = (sem, val)
        seen = self.seen[eng]
        for key, (sem, val) in best.items():
            if seen.get(key, 0) < val:
                self.engs[eng].wait_ge(sem.h, val)
                seen[key] = val
                self.nwait += 1

    def _record(self, tok, reads, writes):
        for r in reads:
            key = id(tok[0])
            if key not in r.r or r.r[key][1] < tok[1]:
                r.r[key] = tok
        for w in writes:
            w.w = tok
            w.r = {}

    def op(self, eng, fn, reads=(), writes=(), inc=True):
        self._wait(eng, self._deps(eng, reads, writes))
        ins = fn(self.engs[eng])
        s = self.esem[eng]
        if inc:
            s.n += 1
            ins.then_inc(s.h, 1)
            tok = (s, s.n, eng)
        else:
            tok = (s, s.n + 1, eng)
        self._record(tok, reads, writes)
        return ins

    def dma(self, q, out, in_, sem, reads=(), writes=(), **kw):
        qn = "dma_" + q
        self._wait(q, self._deps(qn, reads, writes))
        ins = self.engs[q].dma_start(out=out, in_=in_, **kw)
        sem.n += 16
        ins.then_inc(sem.h, 16)
        self._record((sem, sem.n, qn), reads, writes)
        return ins

    def bank(self):
        b = self.bank_i
        self.bank_i = (self.bank_i + 1) % 8
        return b

    def half(self):
        b = self.half_i * 4
        self.half_i ^= 1
        self.bank_i = (b + 4) % 8
        return b

    def barrier(self):
        self.nc.all_engine_barrier()


def build(debug=False, nseq=2, stages=99):
    nc = bass.Bass("TRN2", target_bir_lowering=False)
    k = K(nc)
    dbg = {}

    xT = nc.dram_tensor("xT", [2, D, S], F32, kind="ExternalInput").ap()
    w_in = nc.dram_tensor("w_in", [D, PROJ], F32, kind="ExternalInput").ap()
    w_out = nc.dram_tensor("w_out", [D, D], F32, kind="ExternalInput").ap()
    w_up = nc.dram_tensor("w_up", [D, 2 * DFF], F32, kind="ExternalInput").ap()
    w_down = nc.dram_tensor("w_down", [DFF, D], F32, kind="ExternalInput").ap()
    vecs_d = nc.dram_tensor("vecs", [128, NVEC], F32, kind="ExternalInput").ap()
    cstf_d = nc.dram_tensor("cstf", [128, NCST], F32, kind="ExternalInput").ap()
    sel_d = nc.dram_tensor("sel", [128, 64 * 128], F32, kind="ExternalInput").ap()
    outT = nc.dram_tensor("outT", [2, D, S], F32, kind="ExternalOutput").ap()

    DTB = {F32: 4, BF16: 2}
    arena = {"ptr": ((nc.sbuf_base + 63) // 64) * 64, "top": nc.sbuf_top}

    def A(name, shape, dt, at=None):
        nb = DTB[dt]
        for d_ in shape[1:]:
            nb *= d_
        off = arena["ptr"] if at is None else at
        if at is None:
            arena["ptr"] = ((off + nb + 63) // 64) * 64
            assert arena["ptr"] <= arena["top"], "SBUF arena overflow at %s: %d > %d" % (name, arena["ptr"], arena["top"])
        return nc.alloc_sbuf_tensor_at("s_" + name, shape, dt, offset=off)

    ps = nc.alloc_psum_tensor("ps", [128, 8, 512], F32)
    pb = [k.res("pb%d" % i) for i in range(8)]
    ps_flat = ps[:, :, :].rearrange("p a b -> p (a b)")

    vecs = A("vecs", [128, NVEC], F32)
    cstf = A("cstf", [128, NCST], F32)
    cstb = A("cstb", [128, NCST], BF16)
    maskU = A("maskU", [128, 128], F32)
    tribias = A("tribias", [128, 128], BF16)
    epsc = A("epsc", [128, 1], F32)
    onec = A("onec", [128, 1], F32)
    r_const = k.res("const")
    dsem = k.newsem("d_const")
    osem = [k.newsem("d_out%d" % kc) for kc in range(KC)]
    gsem = k.newsem("d_dbg")
    xsem = [k.newsem("d_x%d" % kc) for kc in range(KC)]

    hT_r = [[k.res("hT%d_%d" % (kc, tb)) for tb in range(NB)] for kc in range(KC)]
    aT = A("aT", [128, KC, S], BF16)
    aT_r = [k.res("aT%d" % tb) for tb in range(NB)]
    MIXT_OFF = arena["ptr"]
    mixT = A("mixT", [128, KC, S], BF16)
    mixT_r = [k.res("mixT%d" % t) for t in range(T)]
    NW = 3
    wr = [A("wr%d" % i, [128, KC, 512], BF16) for i in range(NW)]
    wr_r = [k.res("wr%d" % i) for i in range(NW)]
    wsem = [k.newsem("d_w%d" % i) for i in range(NW)]
    wri = [0]

    def wslot():
        i = wri[0]
        wri[0] = (i + 1) % NW
        return i

    def load_w512(src_cols_ap):
        i = wslot()
        n = src_cols_ap.shape[1]
        k.dma("pool", out=wr[i][:, :, 0:n], in_=src_cols_ap.rearrange("(kc p) n -> p kc n", p=128),
              sem=wsem[i], writes=[wr_r[i]])
        return i

    ARENA0 = arena["ptr"]
    hT = A("hT", [128, KC, S], F32)
    ARENA1 = arena["ptr"]

    def phase_end():
        if debug and gsem.n:
            k.engs["sp"].wait_ge(gsem.h, gsem.n)
        k.barrier()

    k.dma("sp", out=vecs[:], in_=vecs_d, sem=dsem, writes=[r_const])
    k.dma("sp", out=cstf[:], in_=cstf_d, sem=dsem, writes=[r_const])
    k.op("dve", lambda e: e.tensor_copy(out=cstb[:], in_=cstf[:]), reads=[r_const], writes=[r_const])
    k.op("dve", lambda e: e.tensor_scalar(out=maskU[:], in0=cstf[:, C_TRI:C_TRI + 128], scalar1=MD_SCALE,
                                          scalar2=None, op0=ALU.mult), reads=[r_const], writes=[r_const])
    k.op("dve", lambda e: e.tensor_scalar(out=tribias[:], in0=cstf[:, C_TRI:C_TRI + 128], scalar1=-1.0,
                                          scalar2=-NEG, op0=ALU.add, op1=ALU.mult), reads=[r_const], writes=[r_const])
    k.op("dve", lambda e: e.memset(epsc[:], EPS), writes=[r_const])
    k.op("dve", lambda e: e.memset(onec[:], 1.0), writes=[r_const])
    ident_b = cstb[:, C_ID:C_ID + 128]
    ident_f = cstf[:, C_ID:C_ID + 128]
    tri_b = cstb[:, C_TRI:C_TRI + 128]
    tri_f = cstf[:, C_TRI:C_TRI + 128]
    ones_b = cstb[:, C_ONE:C_ONE + 128]
    ones_f = cstf[:, C_ONE:C_ONE + 128]

    def vcol(c):
        return vecs[:, c:c + 1]

    def dump(name, ap, reads):
        if not debug:
            return
        t = nc.dram_tensor(name, list(ap.shape), ap.dtype, kind="ExternalOutput").ap()
        k.dma("sp", out=t, in_=ap, sem=gsem, reads=reads)
        dbg[name] = t

    def tsl(tb):
        return slice(tb * 512, (tb + 1) * 512)

    def norm_phase(src, src_r, gcol0, dst_fn, sq, sq_r, rstd, rstd_r, kc_outer=False):
        b0 = k.half()
        for kc in range(KC):
            i = kc % 2
            k.op("act", lambda e: e.activation(out=sq[i][:], in_=src[:, kc, :], func=AF.Square),
                 reads=src_r[kc], writes=[sq_r[i]])
            for tb in range(NB):
                k.op("pe", lambda e: e.matmul(out=ps[:, b0 + tb, :], lhsT=ones_b, rhs=sq[i][:, tsl(tb)],
                                              start=(kc == 0), stop=(kc == KC - 1)),
                     reads=[sq_r[i], r_const], writes=[pb[b0 + tb]], inc=(tb == NB - 1))
        for tb in range(NB):
            k.op("act", lambda e: e.activation(out=rstd[:, tsl(tb)], in_=ps[:, b0 + tb, :], func=AF.Ln,
                                               bias=epsc[:, 0:1], scale=1.0 / D),
                 reads=[pb[b0 + tb], r_const], writes=[rstd_r[tb]])
            k.op("act", lambda e: e.activation(out=rstd[:, tsl(tb)], in_=rstd[:, tsl(tb)], func=AF.Exp, scale=-0.5),
                 reads=[rstd_r[tb]], writes=[rstd_r[tb]])
        if kc_outer:
            for kc in range(KC):
                for tb in range(NB):
                    dst_fn(kc, tb, gcol0)
        else:
            for tb in range(NB):
                for kc in range(KC):
                    dst_fn(kc, tb, gcol0)

    def proj_tok(t, w_i, ncols, bnk, extra_reads=()):
        for kc in range(KC):
            k.op("pe", lambda e: e.matmul(out=ps[:, bnk, 0:ncols], lhsT=aT[:, kc, t * 128:(t + 1) * 128],
                                          rhs=wr[w_i][:, kc, 0:ncols], start=(kc == 0), stop=(kc == KC - 1)),
                 reads=[aT_r[t // 4], wr_r[w_i]], writes=[pb[bnk]], inc=(kc == KC - 1))

    def proj_fm(w_i, j, b0):
        for tb in range(NB):
            for kc in range(KC):
                k.op("pe", lambda e: e.matmul(out=ps[:, b0 + tb, :], lhsT=wr[w_i][:, kc, j * 128:(j + 1) * 128],
                                              rhs=aT[:, kc, tsl(tb)], start=(kc == 0), stop=(kc == KC - 1)),
                     reads=[aT_r[tb], wr_r[w_i]], writes=[pb[b0 + tb]], inc=(kc == KC - 1))

    def conv_fm(b0, taps, bias_c, acc, acc_r, halo=True):
        base = b0 * 512
        for tb in range(NB):
            k.op("act", lambda e: e.activation(out=acc[:, tsl(tb)], in_=ps[:, b0 + tb, :], func=AF.Identity,
                                               bias=vcol(bias_c), scale=vcol(taps[0])),
                 reads=[pb[b0 + tb], r_const], writes=[acc_r[tb]])
            for sh in range(1, len(taps)):
                lo = tb * 512
                if tb == 0:
                    k.op("dve", lambda e: e.scalar_tensor_tensor(
                        out=acc[:, sh:512], in0=ps_flat[:, base:base + 512 - sh], scalar=vcol(taps[sh]),
                        in1=acc[:, sh:512], op0=ALU.mult, op1=ALU.add),
                        reads=[pb[b0], r_const, acc_r[0]], writes=[acc_r[0]])
                else:
                    k.op("dve", lambda e: e.scalar_tensor_tensor(
                        out=acc[:, lo:lo + 512], in0=ps_flat[:, base + lo - sh:base + lo + 512 - sh],
                        scalar=vcol(taps[sh]), in1=acc[:, lo:lo + 512], op0=ALU.mult, op1=ALU.add),
                        reads=[pb[b0 + tb - 1], pb[b0 + tb], r_const, acc_r[tb]], writes=[acc_r[tb]])

    p1a_pre = []
    for s in range(nseq):
        dd = debug and s == 0
        arena["ptr"] = ARENA1
        sq = [A("sq%d_%d" % (s, i), [128, S], BF16) for i in range(2)]
        sq_r = [k.res("sq%d" % i) for i in range(2)]
        rstd = A("rstd%d" % s, [128, S], F32)
        rstd_r = [k.res("rstd%d" % tb) for tb in range(NB)]
        for kc in range(KC):
            k.dma("sp", out=hT[:, kc, :], in_=xT[s, kc * 128:(kc + 1) * 128, :], sem=xsem[kc], writes=hT_r[kc])

        def dst_a(kc, tb, g0):
            k.op("dve", lambda e: e.scalar_tensor_tensor(out=aT[:, kc, tsl(tb)], in0=hT[:, kc, tsl(tb)],
                                                         scalar=vcol(g0 + kc), in1=rstd[:, tsl(tb)],
                                                         op0=ALU.mult, op1=ALU.mult),
                 reads=[hT_r[kc][tb], rstd_r[tb], r_const], writes=[aT_r[tb]])

        norm_phase(hT, hT_r, V_G1, dst_a, sq, sq_r, rstd, rstd_r)
        if dd:
            dump("d_aT", aT[:, :, :], aT_r)
        if stages <= 0:
            break

        phase_end()
        arena["ptr"] = ARENA0
        QT = A("QT%d" % s, [128, 8, S], BF16)
        KT = A("KT%d" % s, [128, 4, S], BF16)
        QT_r = [k.res("QT%d" % tb) for tb in range(NB)]
        KT_r = [k.res("KT%d" % tb) for tb in range(NB)]
        Vx = A("Vx%d" % s, [128, T, 8, 65], BF16)
        Vx_r = [k.res("Vx%d" % t) for t in range(T)]
        ksum = A("ksum%d" % s, [128, 4, 8], F32)
        ksumb = A("ksumb%d" % s, [128, 4, 8], BF16)
        ksum_r = k.res("ksum")
        maskT = A("maskT%d" % s, [128, S], BF16)
        maskT_r = [k.res("maskT%d" % t) for t in range(T)]
        selb = A("selb%d" % s, [128, 64, 128], BF16)
        sel_r = k.res("sel")
        ssem = k.newsem("d_sel%d" % s)
        if p1a_pre:
            wq, wk, wv = p1a_pre.pop()
        else:
            wq = load_w512(w_in[:, 0:512])
            wk = load_w512(w_in[:, 512:1024])
            wv = load_w512(w_in[:, 1024:1536])
        k.op("pool", lambda e: e.memset(Vx[:, :, :, 64:65], 1.0), writes=Vx_r)
        for j in range(4):
            k.op("pool", lambda e: e.memset(QT[64:128, 2 * j, :], 0.0), writes=QT_r)
            k.op("pool", lambda e: e.memset(QT[0:64, 2 * j + 1, :], 0.0), writes=QT_r)

        for q4 in range(4):
            k.dma("pool", out=selb[:, q4 * 16:(q4 + 1) * 16, :],
                  in_=sel_d[:, q4 * 2048:(q4 + 1) * 2048].rearrange("p (a b) -> p a b", b=128),
                  sem=ssem, writes=[sel_r])
        for j in range(4):
            b0 = k.half()
            proj_fm(wq, j, b0)
            for tb in range(NB):
                k.op("dve", lambda e: e.tensor_scalar(out=QT[0:64, 2 * j, tsl(tb)], in0=ps[0:64, b0 + tb, :], scalar1=0.125,
                                                      scalar2=None, op0=ALU.mult),
                     reads=[pb[b0 + tb]], writes=[QT_r[tb]])
                k.op("dve", lambda e: e.tensor_scalar(out=QT[64:128, 2 * j + 1, tsl(tb)], in0=ps[64:128, b0 + tb, :],
                                                      scalar1=0.125, scalar2=None, op0=ALU.mult),
                     reads=[pb[b0 + tb]], writes=[QT_r[tb]])
        for j in range(4):
            b0 = k.half()
            proj_fm(wk, j, b0)
            for tb in range(NB):
                for hb in range(2):
                    blk = tb * 2 + hb
                    k.op("act", lambda e: e.activation(out=KT[:, j, blk * 256:(blk + 1) * 256],
                                                       in_=ps[:, b0 + tb, hb * 256:(hb + 1) * 256], func=AF.Copy,
                                                       accum_out=ksum[:, j, blk:blk + 1]),
                         reads=[pb[b0 + tb]], writes=[KT_r[tb], ksum_r])
        k.op("dve", lambda e: e.tensor_copy(out=ksumb[:], in_=ksum[:]), reads=[ksum_r], writes=[ksum_r])
        mb = [A("mb%d_%d" % (s, i), [128, 16, 8], F32) for i in range(2)]
        mb_r = [k.res("mb%d" % i) for i in range(2)]
        Gs = A("Gs%d" % s, [128, 8, 8], F32)
        cmp_t = A("cmp%d" % s, [128, 8, 8, 8], F32)
        rank = A("rank%d" % s, [128, 8, 8], F32)
        gs_r = k.res("gs")

        def vproj(t):
            bnk = k.bank()
            proj_tok(t, wv, 512, bnk)
            k.op("dve", lambda e: e.tensor_copy(out=Vx[:, t, :, 0:64],
                                                in_=ps[:, bnk, :].rearrange("p (h d) -> p h d", d=64)),
                 reads=[pb[bnk]], writes=[Vx_r[t]])

        def mask_front(t):
            blk = t // 2
            m = mb[t % 2]
            mr = mb_r[t % 2]
            k.op("dve", lambda e: e.memset(m[:], NEG), writes=[mr])
            if blk <= 3:
                k.op("dve", lambda e: e.memset(m[:, :, 0:blk], 0.0), writes=[mr])
                return
            bnk = k.bank()
            for h in range(8):
                j = h // 2
                k.op("pe", lambda e: e.matmul(out=ps[:, bnk, h * 8:(h + 1) * 8],
                                              lhsT=QT[:, h, t * 128:(t + 1) * 128],
                                              rhs=ksumb[:, j, :], start=True, stop=True),
                     reads=[QT_r[t // 4], ksum_r], writes=[pb[bnk]], inc=(h == 7))
            k.op("dve", lambda e: e.tensor_copy(out=Gs[:], in_=ps[:, bnk, 0:64].rearrange("p (h n) -> p h n", n=8)),
                 reads=[pb[bnk]], writes=[gs_r])
            k.op("dve", lambda e: e.tensor_tensor(
                out=cmp_t[:, :, 0:blk, 0:blk],
                in0=Gs[:, :, 0:blk].unsqueeze(2).to_broadcast([128, 8, blk, blk]),
                in1=Gs[:, :, 0:blk].unsqueeze(3).to_broadcast([128, 8, blk, blk]), op=ALU.is_gt),
                reads=[gs_r], writes=[gs_r])
            k.op("dve", lambda e: e.tensor_reduce(out=rank[:, :, 0:blk], in_=cmp_t[:, :, 0:blk, 0:blk],
                                                  axis=AX.X, op=ALU.add), reads=[gs_r], writes=[gs_r])
            for c2 in range(2):
                k.op("dve", lambda e: e.tensor_scalar(out=m[:, c2 * 8:(c2 + 1) * 8, 0:blk], in0=rank[:, :, 0:blk],
                                                      scalar1=2.5, scalar2=NEG, op0=ALU.is_gt, op1=ALU.mult),
                     reads=[gs_r], writes=[mr])

        def mask_back(t):
            m = mb[t % 2]
            mr = mb_r[t % 2]
            bnk = k.bank()
            k.op("pe", lambda e: e.transpose(out=ps[:, bnk, 0:128], in_=m[:].rearrange("p a b -> p (a b)"),
                                             identity=ident_f),
                 reads=[mr, r_const], writes=[pb[bnk]])
            k.op("act", lambda e: e.activation(out=maskT[:, t * 128:(t + 1) * 128], in_=ps[:, bnk, 0:128], func=AF.Copy),
                 reads=[pb[bnk]], writes=[maskT_r[t]])

        for t in range(T + 1):
            if 2 <= t < T:
                mask_front(t)
            if t < T:
                vproj(t)
            if 2 <= t - 1 < T:
                mask_back(t - 1)
        if dd:
            dump("d_QT", QT[:, :, :], QT_r)
            dump("d_KT", KT[:, :, :], KT_r)
            dump("d_Vx", Vx[:, :, :, :], Vx_r)
        if dd:
            dump("d_maskT", maskT[:, 256:], maskT_r[2:])

        if stages <= 0.7:
            break
        p1b_pre = [load_w512(w_in[:, 3584:3592]), load_w512(w_in[:, 1536:2048]), load_w512(w_in[:, 2048:2560])]
        NPT = 3
        LA = 2
        PT = [A("PT%d_%d" % (s, i), [128, 512], BF16) for i in range(NPT)]
        PT_r = [k.res("PT%d" % i) for i in range(NPT)]
        attTok = [A("attTok%d_%d" % (s, i), [128, 2, 512], F32) for i in range(2)]
        attTok_r = [[k.res("attTok%d_%d" % (i, q)) for q in range(2)] for i in range(2)]
        rd = A("rd%d" % s, [128, 4], F32)
        rd_r = k.res("rd")
        sqt = [A("sqt%d_%d" % (s, i), [128, 512], F32) for i in range(2)]
        ssn = [A("ssn%d_%d" % (s, i), [128, 8], F32) for i in range(2)]
        nrm_r = [k.res("nrm%d" % i) for i in range(2)]
        attb = [A("attb%d_%d" % (s, i), [128, 512], BF16) for i in range(2)]
        attb_r = [k.res("attb%d" % i) for i in range(2)]
        units = [(i, h, n) for i in range(8) for h in range(8) for n in range(i + 1)]

        def att_A(idx):
            i, h, n = units[idx]
            j = h // 2
            q0 = i * 256
            bs = 4 + idx % 3
            p, pr = PT[idx % NPT], PT_r[idx % NPT]
            diag = (n == i)
            for half in range(2):
                kt = 2 * n + half
                qlo = 128 if (diag and half == 1) else 0
                c0 = half * 256
                k.op("pe", lambda e: e.matmul(out=ps[:, bs, c0 + qlo:c0 + 256], lhsT=KT[:, j, kt * 128:(kt + 1) * 128],
                                              rhs=QT[:, h, q0 + qlo:q0 + 256], start=True, stop=False),
                     reads=[KT_r[kt // 4], QT_r[q0 // 512]], writes=[pb[bs]], inc=False)
                if diag:
                    k.op("pe", lambda e: e.matmul(out=ps[:, bs, c0 + qlo:c0 + qlo + 128], lhsT=ident_b, rhs=tribias[:],
                                                  start=False, stop=True),
                         reads=[r_const], writes=[pb[bs]], inc=(half == 1))
                if not diag:
                    k.op("pe", lambda e: e.matmul(out=ps[:, bs, c0:c0 + 256], lhsT=selb[:, h * 8 + n, :],
                                                  rhs=maskT[:, q0:q0 + 256], start=False, stop=True),
                         reads=[sel_r, maskT_r[2 * i], maskT_r[2 * i + 1]], writes=[pb[bs]], inc=(half == 1))
            k.op("act", lambda e: e.activation(out=p[:], in_=ps[:, bs, :], func=AF.Exp),
                 reads=[pb[bs]], writes=[pr])

        def att_B(idx):
            i, h, n = units[idx]
            nkt = 2 * i + 2
            p, pr = PT[idx % NPT], PT_r[idx % NPT]
            ih = i * 8 + h
            bo = [(ih % 2) * 2, (ih % 2) * 2 + 1]
            at, at_r = attTok[i % 2], attTok_r[i % 2]
            diag = (n == i)
            for half in range(2):
                kt = 2 * n + half
                for qt in range(2):
                    if diag and half == 1 and qt == 0:
                        continue
                    first = (kt == 0)
                    last = (kt == nkt - 1) if qt == 1 else (kt == nkt - 2)
                    c0 = half * 256 + qt * 128
                    k.op("pe", lambda e: e.matmul(out=ps[:, bo[qt], 0:65], lhsT=p[:, c0:c0 + 128],
                                                  rhs=Vx[:, kt, h, :], start=first, stop=last),
                         reads=[pr, Vx_r[kt]], writes=[pb[bo[qt]]], inc=last)
            if not diag:
                return
            for qt in range(2):
                k.op("dve", lambda e: e.reciprocal(out=rd[:, qt:qt + 1], in_=ps[:, bo[qt], 64:65]),
                     reads=[pb[bo[qt]]], writes=[rd_r])
                k.op("dve", lambda e: e.tensor_scalar(out=at[:, qt, h * 64:(h + 1) * 64], in0=ps[:, bo[qt], 0:64],
                                                      scalar1=rd[:, qt:qt + 1], scalar2=None, op0=ALU.mult),
                     reads=[pb[bo[qt]], rd_r], writes=[at_r[qt]])
            if h != 7:
                return
            for qt in range(2):
                t = 2 * i + qt

                def st0(qt=qt):
                    k.op("act", lambda e: e.activation(out=sqt[qt][:], in_=at[:, qt, :], func=AF.Square),
                         reads=[at_r[qt]], writes=[nrm_r[qt]])

                def st1(qt=qt):
                    k.op("dve", lambda e: e.tensor_reduce(out=ssn[qt][:], in_=sqt[qt][:].rearrange("p (h d) -> p h d", d=64),
                                                          axis=AX.X, op=ALU.add), reads=[nrm_r[qt]], writes=[nrm_r[qt]])
                    k.op("act", lambda e: e.activation(out=ssn[qt][:], in_=ssn[qt][:], func=AF.Ln, bias=epsc[:, 0:1],
                                                       scale=1.0 / 64), reads=[nrm_r[qt], r_const], writes=[nrm_r[qt]])
                    k.op("act", lambda e: e.activation(out=ssn[qt][:], in_=ssn[qt][:], func=AF.Exp, scale=-0.5),
                         reads=[nrm_r[qt]], writes=[nrm_r[qt]])

                def st2(qt=qt):
                    k.op("dve", lambda e: e.tensor_tensor(out=attb[qt][:].rearrange("p (h d) -> p h d", d=64),
                                                          in0=at[:, qt, :].rearrange("p (h d) -> p h d", d=64),
                                                          in1=ssn[qt][:].unsqueeze(2).to_broadcast([128, 8, 64]), op=ALU.mult),
                         reads=[at_r[qt], nrm_r[qt]], writes=[attb_r[qt]])

                def st3(qt=qt, t=t):
                    bnk = 7
                    psb = ps[:, bnk, :].bitcast(BF16)
                    for jj in range(4):
                        k.op("pe", lambda e: e.transpose(out=psb[:, jj * 128:(jj + 1) * 128],
                                                         in_=attb[qt][:, jj * 128:(jj + 1) * 128], identity=ident_b),
                             reads=[attb_r[qt], r_const], writes=[pb[bnk]], inc=(jj == 3))
                    for jj in range(4):
                        k.op("dve", lambda e: e.tensor_scalar(out=mixT[:, jj, t * 128:(t + 1) * 128],
                                                              in0=psb[:, jj * 128:(jj + 1) * 128], scalar1=vcol(V_GATT + jj),
                                                              scalar2=None, op0=ALU.mult),
                             reads=[pb[bnk], r_const], writes=[mixT_r[t]])

                for kk_, f_ in enumerate((st0, st1, st2, st3)):
                    asched.setdefault(idx + 1 + 2 * kk_ + qt, []).append(f_)

        asched = {}
        for idx in range(len(units) + LA):
            if idx < len(units):
                att_A(idx)
            if idx >= LA:
                att_B(idx - LA)
                for f_ in asched.pop(idx - LA, []):
                    f_()
        for key_ in sorted(asched):
            for f_ in asched[key_]:
                f_()
        if dd:
            dump("d_mixA", mixT[:, 0:4, :], mixT_r)
        if stages <= 1:
            break
        phase_end()
        NPRE = 2
        arena["ptr"] = ARENA0 + NPRE * S * 4
        for kc in range(NPRE):
            k.dma("sp", out=hT[:, kc, :], in_=xT[s, kc * 128:(kc + 1) * 128, :], sem=xsem[kc], writes=hT_r[kc])
        mqkT = A("mqkT%d" % s, [128, 8, S], BF16)
        mqk_r = [[k.res("mqk%d_%d" % (c_, tb)) for tb in range(NB)] for c_ in range(8)]
        mkTok = A("mkTok%d" % s, [128, T, 4, 128], BF16)
        mkTok_r = [k.res("mkTok%d" % t) for t in range(T)]
        mvx = A("mvx%d" % s, [128, T, 4, 129], BF16)
        mvx_r = [k.res("mvx%d" % t) for t in range(T)]
        acc = [A("macc%d_%d" % (s, i), [128, S], F32) for i in range(1)]
        acc_r = [[k.res("macc%d_%d" % (i, tb)) for tb in range(NB)] for i in range(1)]
        gsb = A("gsb%d" % s, [128, T, 8], F32)
        ee = A("ee%d" % s, [128, T, 4], F32)
        lf = A("lf%d" % s, [128, T * 4], F32)
        CTs = A("CTs%d" % s, [128, T * 4], F32)
        rr = A("rr%d" % s, [128, T * 4], F32)
        wgt = A("wgt%d" % s, [128, T * 4], F32)
        bnd = A("bnd%d" % s, [128, T * 4], F32)
        alp = A("alp%d" % s, [128, T * 4], F32)
        alp2 = A("alp2_%d" % s, [128, T * 4], F32)
        gate_r = k.res("gate")
        k.op("pool", lambda e: e.memset(mvx[:, :, :, 128:129], 1.0), writes=mvx_r)

        wg_i = p1b_pre[0]
        bg = k.bank()
        for t in range(T):
            for kc in range(KC):
                k.op("pe", lambda e: e.matmul(out=ps[:, bg, t * 8:(t + 1) * 8], lhsT=aT[:, kc, t * 128:(t + 1) * 128],
                                              rhs=wr[wg_i][:, kc, 0:8], start=(kc == 0), stop=(kc == KC - 1)),
                     reads=[aT_r[t // 4], wr_r[wg_i]], writes=[pb[bg]], inc=(kc == KC - 1 and t == T - 1))
        k.op("dve", lambda e: e.tensor_tensor(out=gsb[:], in0=ps[:, bg, 0:128].rearrange("p (t g) -> p t g", g=8),
                                              in1=vecs[:, V_BG:V_BG + 8].unsqueeze(1).to_broadcast([128, T, 8]), op=ALU.add),
             reads=[pb[bg], r_const], writes=[gate_r])
        k.op("act", lambda e: e.activation(out=ee[:], in_=gsb[:, :, 4:8], func=AF.Exp, scale=-1.0),
             reads=[gate_r], writes=[gate_r])
        k.op("act", lambda e: e.activation(out=ee[:], in_=ee[:], func=AF.Ln, bias=onec[:, 0:1]),
             reads=[gate_r, r_const], writes=[gate_r])
        k.op("dve", lambda e: e.tensor_scalar(out=lf[:], in0=ee[:].rearrange("p t g -> p (t g)"), scalar1=-1.0, scalar2=None,
                                              op0=ALU.mult), reads=[gate_r], writes=[gate_r])
        bb = k.bank()
        k.op("pe", lambda e: e.matmul(out=ps[:, bb, 0:64], lhsT=tri_f, rhs=lf[:], start=True, stop=True),
             reads=[gate_r, r_const], writes=[pb[bb]], inc=False)
        k.op("pe", lambda e: e.matmul(out=ps[:, bb, 64:128], lhsT=ones_f, rhs=lf[:], start=True, stop=True),
             reads=[gate_r, r_const], writes=[pb[bb]])
        k.op("dve", lambda e: e.tensor_copy(out=CTs[:], in_=ps[:, bb, 64:128]), reads=[pb[bb]], writes=[gate_r])
        k.op("dve", lambda e: e.tensor_tensor(out=rr[:], in0=CTs[:], in1=ps[:, bb, 0:64], op=ALU.subtract),
             reads=[pb[bb], gate_r], writes=[gate_r])
        k.op("dve", lambda e: e.tensor_tensor(out=wgt[:].rearrange("p (t g) -> p t g", g=4), in0=gsb[:, :, 0:4],
                                              in1=rr[:].rearrange("p (t g) -> p t g", g=4), op=ALU.add),
             reads=[gate_r], writes=[gate_r])
        k.op("act", lambda e: e.activation(out=wgt[:], in_=wgt[:], func=AF.Exp), reads=[gate_r], writes=[gate_r])
        k.op("act", lambda e: e.activation(out=bnd[:], in_=rr[:], func=AF.Exp), reads=[gate_r], writes=[gate_r])
        k.op("act", lambda e: e.activation(out=alp[:], in_=CTs[:], func=AF.Exp), reads=[gate_r], writes=[gate_r])
        k.op("dve", lambda e: e.tensor_scalar(out=alp2[:], in0=alp[:], scalar1=MD_SCALE, scalar2=None, op0=ALU.mult),
             reads=[gate_r], writes=[gate_r])

        for grp in range(2):
            w_i = p1b_pre[1 + grp]
            for j in range(4):
                cj = grp * 4 + j
                b0 = k.half()
                proj_fm(w_i, j, b0)
                ai = 0
                conv_fm(b0, [V_MCW + 3 * 8 + cj, V_MCW + 2 * 8 + cj, V_MCW + 1 * 8 + cj, V_MCW + cj], V_MCB + cj,
                        acc[ai], acc_r[ai])
                for tb in range(NB):
                    k.op("act", lambda e: e.activation(out=mqkT[:, cj, tsl(tb)], in_=acc[ai][:, tsl(tb)], func=AF.Silu),
                         reads=[acc_r[ai][tb]], writes=[mqk_r[cj][tb]])
        if dd:
            dump("d_mqkT", mqkT[:, :, :], [r_ for l_ in mqk_r for r_ in l_])
        for t in range(T):
            bnk = k.bank()
            psb = ps[:, bnk, :].bitcast(BF16)
            for h in range(4):
                k.op("pe", lambda e: e.transpose(out=psb[:, h * 128:(h + 1) * 128], in_=mqkT[:, 4 + h, t * 128:(t + 1) * 128],
                                                 identity=ident_b),
                     reads=[mqk_r[4 + h][t // 4], r_const], writes=[pb[bnk]], inc=(h == 3))
            k.op("act", lambda e: e.activation(out=mkTok[:, t, :, :].rearrange("p h d -> p (h d)"), in_=psb[:, 0:512],
                                               func=AF.Copy), reads=[pb[bnk]], writes=[mkTok_r[t]])
        wv_i = load_w512(w_in[:, 2560:3072])
        for t in range(T):
            bnk = k.bank()
            proj_tok(t, wv_i, 512, bnk)
            k.op("dve", lambda e: e.tensor_copy(out=mvx[:, t, :, 0:128],
                                                in_=ps[:, bnk, :].rearrange("p (h d) -> p h d", d=128)),
                 reads=[pb[bnk]], writes=[mvx_r[t]])
        wo_i = load_w512(w_in[:, 3072:3584])
        wout_i = [load_w512(w_out[:, 0:512]), load_w512(w_out[:, 512:1024])]

        U = [A("U%d_%d" % (s, h), [128, 129], F32) for h in range(4)]
        U_r = [k.res("U%d" % h) for h in range(4)]
        Cbf = [A("Cbf%d_%d" % (s, h), [128, 129], BF16) for h in range(4)]
        Cbf_r = [k.res("Cbf%d" % h) for h in range(4)]
        NPM = 4
        PTm = [A("PTm%d_%d" % (s, i), [128, 128], BF16) for i in range(NPM)]
        PTm_r = [k.res("PTm%d" % i) for i in range(NPM)]
        mvw = [A("mvw%d_%d" % (s, i), [128, 129], BF16) for i in range(NPM)]
        mvw_r = [k.res("mvw%d" % i) for i in range(NPM)]
        nraw = [A("nraw%d_%d" % (s, i), [128, 4, 129], F32) for i in range(2)]
        nraw_r = [k.res("nraw%d" % i) for i in range(2)]
        d4 = A("d4_%d" % s, [128, 4], F32)
        rc4 = A("rc4_%d" % s, [128, 4], F32)
        d4_r = k.res("d4")
        S4 = A("S4_%d" % s, [128, 4], F32)
        s4_r = k.res("s4")
        t4 = A("t4_%d" % s, [128, 4], F32)
        t4_r = k.res("t4")
        f4 = A("f4_%d" % s, [128, 4], F32)
        f4_r = k.res("f4")
        sig = [A("sig%d_%d" % (s, i), [128, 512], F32) for i in range(3)]
        sig_r = [k.res("sig%d" % i) for i in range(3)]
        sq2 = A("sq2_%d" % s, [128, 512], F32)
        ss4 = A("ss4_%d" % s, [128, 4], F32)
        tmpn = A("tmpn%d" % s, [128, 512], F32)
        nrm2_r = k.res("nrm2")
        mixb = A("mixb%d" % s, [128, 512], BF16)
        mixb_r = k.res("mixb")
        munits = [(c, h) for c in range(T) for h in range(4)]
        mvwc = [A("mvwc%d_%d" % (s, i), [128, 4, 129], BF16) for i in range(2)]
        mvwc_r = [k.res("mvwc%d" % i) for i in range(2)]

        def mk_mvw(c):
            k.op("dve", lambda e: e.tensor_tensor(out=mvwc[c % 2][:], in0=mvx[:, c, :, :],
                                                  in1=wgt[:, c * 4:(c + 1) * 4].unsqueeze(2).to_broadcast([128, 4, 129]),
                                                  op=ALU.mult), reads=[mvx_r[c], gate_r], writes=[mvwc_r[c % 2]])

        mk_mvw(0)
        mk_mvw(1)

        def ml_A(u):
            c, h = munits[u]
            csl = slice(c * 128, (c + 1) * 128)
            col = c * 4 + h
            pm, pm_r, vw, vw_r = PTm[u % NPM], PTm_r[u % NPM], mvw[u % NPM], mvw_r[u % NPM]
            bs = u % 5
            if h == 0 and c >= 1 and c + 1 < T:
                mk_mvw(c + 1)
            vw, vw_r = mvwc[c % 2][:, h, :], mvwc_r[c % 2]
            k.op("pe", lambda e: e.matmul(out=ps[:, bs, 0:128], lhsT=mqkT[:, 4 + h, csl], rhs=mqkT[:, h, csl],
                                          start=True, stop=True),
                 reads=[mqk_r[4 + h][c // 4], mqk_r[h][c // 4]], writes=[pb[bs]])
            k.op("pe", lambda e: e.matmul(out=ps[:, bs, 128:257], lhsT=mkTok[:, c, h, :], rhs=vw, start=True, stop=True),
                 reads=[mkTok_r[c], vw_r], writes=[pb[bs]])
            k.op("dve", lambda e: e.scalar_tensor_tensor(out=pm[:], in0=ps[:, bs, 0:128], scalar=wgt[:, col:col + 1],
                                                         in1=maskU[:], op0=ALU.mult, op1=ALU.mult),
                 reads=[pb[bs], gate_r, r_const], writes=[pm_r])
            if h == 0:
                bm = 5 + c % 2
                proj_tok(c, wo_i, 512, bm)
                k.op("act", lambda e: e.activation(out=sig[c % 3][:], in_=ps[:, bm, :], func=AF.Exp, scale=-1.0),
                     reads=[pb[bm]], writes=[sig_r[c % 3]])

        def ml_B(u):
            c, h = munits[u]
            csl = slice(c * 128, (c + 1) * 128)
            col = c * 4 + h
            pm, pm_r = PTm[u % NPM], PTm_r[u % NPM]
            bs = u % 5
            bn = bs
            nr, nr_r = nraw[c % 2], nraw_r[c % 2]
            k.op("pe", lambda e: e.matmul(out=ps[:, bn, 257:386], lhsT=pm[:], rhs=mvx[:, c, h, :], start=True, stop=(c == 0)),
                 reads=[pm_r, mvx_r[c]], writes=[pb[bn]], inc=(c == 0))
            if c > 0:
                k.op("pe", lambda e: e.matmul(out=ps[:, bn, 257:386], lhsT=mqkT[:, h, csl], rhs=Cbf[h][:], start=False,
                                              stop=True),
                     reads=[mqk_r[h][c // 4], Cbf_r[h]], writes=[pb[bn]])
            k.op("dve", lambda e: e.tensor_copy(out=nr[:, h, :], in_=ps[:, bn, 257:386]),
                 reads=[pb[bn]], writes=[nr_r])
            if c == 0:
                k.op("dve", lambda e: e.tensor_copy(out=U[h][:], in_=ps[:, bs, 128:257]), reads=[pb[bs]], writes=[U_r[h]])
            else:
                k.op("dve", lambda e: e.scalar_tensor_tensor(out=U[h][:], in0=U[h][:], scalar=alp[:, col:col + 1],
                                                             in1=ps[:, bs, 128:257], op0=ALU.mult, op1=ALU.add),
                     reads=[pb[bs], gate_r, U_r[h]], writes=[U_r[h]])
            if c < T - 1:
                k.op("act", lambda e: e.activation(out=Cbf[h][:], in_=U[h][:], func=AF.Copy, scale=alp2[:, col + 4:col + 5]),
                     reads=[U_r[h], gate_r], writes=[Cbf_r[h]])
            if h != 3:
                return
            sg, sg_r = sig[c % 3], sig_r[c % 3]

            def mt0():
                k.op("dve", lambda e: e.scalar_tensor_tensor(out=d4[:], in0=nr[:, :, 128], scalar=-1.0,
                                                             in1=bnd[:, c * 4:c * 4 + 4], op0=ALU.mult, op1=ALU.max),
                     reads=[nr_r, gate_r], writes=[d4_r])
                k.op("dve", lambda e: e.tensor_tensor(out=d4[:], in0=d4[:], in1=nr[:, :, 128], op=ALU.max),
                     reads=[nr_r, d4_r], writes=[d4_r])
                k.op("dve", lambda e: e.reciprocal(out=rc4[:], in_=d4[:]), reads=[d4_r], writes=[d4_r])
                for h2 in range(4):
                    k.op("act", lambda e: e.activation(out=sq2[:, 0:128], in_=nr[:, h2, 0:128], func=AF.Square,
                                                       accum_out=S4[:, h2:h2 + 1]), reads=[nr_r], writes=[s4_r])

            def mt1():
                k.op("dve", lambda e: e.tensor_tensor(out=t4[:], in0=rc4[:], in1=rc4[:], op=ALU.mult),
                     reads=[d4_r], writes=[t4_r])
                k.op("dve", lambda e: e.tensor_tensor(out=t4[:], in0=t4[:], in1=S4[:], op=ALU.mult),
                     reads=[t4_r, s4_r], writes=[t4_r])
                k.op("act", lambda e: e.activation(out=t4[:], in_=t4[:], func=AF.Ln, bias=epsc[:, 0:1], scale=1.0 / 128),
                     reads=[t4_r, r_const], writes=[t4_r])
                k.op("act", lambda e: e.activation(out=t4[:], in_=t4[:], func=AF.Exp, scale=-0.5),
                     reads=[t4_r], writes=[t4_r])
                k.op("dve", lambda e: e.tensor_tensor(out=f4[:], in0=t4[:], in1=rc4[:], op=ALU.mult),
                     reads=[t4_r, d4_r], writes=[f4_r])
                k.op("act", lambda e: e.activation(out=sg[:], in_=sg[:], func=AF.Ln, bias=onec[:, 0:1]),
                     reads=[sg_r, r_const], writes=[sg_r])
                k.op("act", lambda e: e.activation(out=sg[:], in_=sg[:], func=AF.Exp, scale=-1.0),
                     reads=[sg_r], writes=[sg_r])

            def mt2():
                k.op("dve", lambda e: e.tensor_tensor(out=tmpn[:].rearrange("p (h d) -> p h d", d=128), in0=nr[:, :, 0:128],
                                                      in1=f4[:].unsqueeze(2).to_broadcast([128, 4, 128]), op=ALU.mult),
                     reads=[nr_r, f4_r], writes=[nrm2_r])
                k.op("pool", lambda e: e.tensor_tensor(out=mixb[:], in0=tmpn[:], in1=sg[:], op=ALU.mult),
                     reads=[nrm2_r, sg_r], writes=[mixb_r])

            def mt3():
                bt = 7
                psb = ps[:, bt, :].bitcast(BF16)
                for h2 in range(4):
                    k.op("pe", lambda e: e.transpose(out=psb[:, h2 * 128:(h2 + 1) * 128], in_=mixb[:, h2 * 128:(h2 + 1) * 128],
                                                     identity=ident_b), reads=[mixb_r, r_const], writes=[pb[bt]], inc=(h2 == 3))
                for h2 in range(4):
                    if h2 % 2 == 0:
                        k.op("act", lambda e: e.activation(out=mixT[:, 4 + h2, csl], in_=psb[:, h2 * 128:(h2 + 1) * 128],
                                                           func=AF.Copy, scale=vcol(V_GML + h2)),
                             reads=[pb[bt], r_const], writes=[mixT_r[c]])
                    else:
                        k.op("dve", lambda e: e.tensor_scalar(out=mixT[:, 4 + h2, csl], in0=psb[:, h2 * 128:(h2 + 1) * 128],
                                                              scalar1=vcol(V_GML + h2), scalar2=None, op0=ALU.mult),
                             reads=[pb[bt], r_const], writes=[mixT_r[c]])

            for kk_, f_ in enumerate((mt0, mt1, mt2, mt3)):
                msched.setdefault(u + kk_, []).append(f_)

        msched = {}
        MLA = 3
        for u in range(len(munits) + MLA):
            if u < len(munits):
                ml_A(u)
            if u >= MLA:
                ml_B(u - MLA)
                for f_ in msched.pop(u - MLA, []):
                    f_()
        for key_ in sorted(msched):
            for f_ in msched[key_]:
                f_()
        if dd:
            dump("d_mixB", mixT[:, 4:8, :], mixT_r)
        if stages <= 2:
            break
        phase_end()

        arena["ptr"] = ARENA1
        wd = [A("wd%d_%d" % (s, i), [128, 4, D], BF16) for i in range(2)]
        wd_r = [k.res("wd%d" % i) for i in range(2)]
        wdsem = [k.newsem("d_wd%d_%d" % (s, i)) for i in range(2)]
        ARENA_FFN = arena["ptr"]
        groups = ((0, 8), (8, 16), (16, 22))
        subs = [(gi, j0, j1, jb, min(4, j1 - jb)) for gi, (j0, j1) in enumerate(groups) for jb in range(j0, j1, 4)]
        wup = {}

        def issue_wd(gi):
            j0, j1 = groups[gi]
            for jb in range(j0, j1, 4):
                nch = min(4, j1 - jb)
                si = (jb - j0) // 4
                k.dma("pool", out=wd[si][:, 0:nch, :],
                      in_=w_down[jb * 128:(jb + nch) * 128, :].rearrange("(c p) n -> p c n", p=128),
                      sem=wdsem[si], writes=[wd_r[si]])

        def issue_g(sb):
            _, _, _, jb, nch = subs[sb]
            wup[(sb, 0)] = load_w512(w_up[:, jb * 128:(jb + nch) * 128])

        def issue_v(sb):
            _, _, _, jb, nch = subs[sb]
            wup[(sb, 1)] = load_w512(w_up[:, (NFC + jb) * 128:(NFC + jb + nch) * 128])

        sqb = [A("sqb%d_%d" % (s, i), [128, 512], BF16) for i in range(4)]
        sqb_r = [k.res("sqb%d" % i) for i in range(4)]
        rstd2 = A("rstd2_%d" % s, [128, S], F32)
        rstd2_r = [k.res("rstd2_%d" % tb) for tb in range(NB)]
        for kc in range(NPRE, KC):
            k.dma("sp", out=hT[:, kc, :], in_=xT[s, kc * 128:(kc + 1) * 128, :], sem=xsem[kc], writes=hT_r[kc])
        issue_g(0)
        issue_wd(0)
        pend = []

        def norm_mm(dc, tb, si):
            k.op("pe", lambda e: e.matmul(out=ps[:, 4 + tb, :], lhsT=ones_b, rhs=sqb[si][:], start=(dc == 0),
                                          stop=(dc == KC - 1)), reads=[sqb_r[si], r_const], writes=[pb[4 + tb]])

        it = 0
        for dc in range(KC):
            wsl = wout_i[dc // 4]
            for tb in range(NB):
                b = it % 4
                for kc in range(KC):
                    k.op("pe", lambda e: e.matmul(out=ps[:, b, :], lhsT=wr[wsl][:, kc, (dc % 4) * 128:(dc % 4 + 1) * 128],
                                                  rhs=mixT[:, kc, tsl(tb)], start=(kc == 0), stop=(kc == KC - 1)),
                         reads=[wr_r[wsl]] + mixT_r[tb * 4:(tb + 1) * 4], writes=[pb[b]], inc=(kc == KC - 1))
                k.op("dve", lambda e: e.tensor_tensor(out=hT[:, dc, tsl(tb)], in0=ps[:, b, :], in1=hT[:, dc, tsl(tb)],
                                                      op=ALU.add), reads=[pb[b], hT_r[dc][tb]], writes=[hT_r[dc][tb]])
                k.op("act", lambda e: e.activation(out=sqb[it % 4][:], in_=hT[:, dc, tsl(tb)], func=AF.Square),
                     reads=[hT_r[dc][tb]], writes=[sqb_r[it % 4]])
                pend.append((dc, tb, it % 4))
                if len(pend) > 2:
                    norm_mm(*pend.pop(0))
                it += 1
        while pend:
            norm_mm(*pend.pop(0))
        issue_v(0)
        if dd:
            dump("d_hT", hT[:, :, :], [r_ for l_ in hT_r for r_ in l_])
        if stages <= 3:
            break
        for tb in range(NB):
            k.op("act", lambda e: e.activation(out=rstd2[:, tsl(tb)], in_=ps[:, 4 + tb, :], func=AF.Ln,
                                               bias=epsc[:, 0:1], scale=1.0 / D),
                 reads=[pb[4 + tb], r_const], writes=[rstd2_r[tb]])
            k.op("act", lambda e: e.activation(out=rstd2[:, tsl(tb)], in_=rstd2[:, tsl(tb)], func=AF.Exp, scale=-0.5),
                 reads=[rstd2_r[tb]], writes=[rstd2_r[tb]])
        for tb in range(NB):
            for kc in range(KC):
                k.op("dve", lambda e: e.scalar_tensor_tensor(out=aT[:, kc, tsl(tb)], in0=hT[:, kc, tsl(tb)],
                                                             scalar=vcol(V_G2 + kc), in1=rstd2[:, tsl(tb)],
                                                             op0=ALU.mult, op1=ALU.mult),
                     reads=[hT_r[kc][tb], rstd2_r[tb], r_const], writes=[aT_r[tb]])
        phase_end()

        arena["ptr"] = ARENA_FFN
        G = A("G%d" % s, [128, 8, S], BF16, at=MIXT_OFF)
        G_r = [[k.res("G%d_%d" % (jj, tb)) for tb in range(NB)] for jj in range(8)]
        accg = A("accg%d" % s, [128, S], F32)
        accg_r = [k.res("accg%d" % tb) for tb in range(NB)]
        accv = A("accv%d" % s, [128, S], F32)
        accv_r = [k.res("accv%d" % tb) for tb in range(NB)]
        sgt = A("sgt%d" % s, [128, S], F32)
        sgt_r = [k.res("sgt%d" % tb) for tb in range(NB)]
        for sb, (gi, j0, j1, jb, nch) in enumerate(subs):
            if sb + 1 < len(subs):
                issue_g(sb + 1)
            wg_i, wv_i = wup[(sb, 0)], wup[(sb, 1)]
            for jj in range(nch):
                j = jb + jj
                jg = j - j0
                b0 = k.half()
                proj_fm(wg_i, jj, b0)
                conv_fm(b0, [V_FCW + 2 * 44 + j, V_FCW + 44 + j, V_FCW + j], V_FCB + j, accg, accg_r)
                for tb in range(NB):
                    k.op("act", lambda e: e.activation(out=sgt[:, tsl(tb)], in_=accg[:, tsl(tb)], func=AF.Silu),
                         reads=[accg_r[tb]], writes=[sgt_r[tb]])
                b1 = k.half()
                proj_fm(wv_i, jj, b1)
                jv = NFC + j
                conv_fm(b1, [V_FCW + 2 * 44 + jv, V_FCW + 44 + jv, V_FCW + jv], V_FCB + jv, accv, accv_r)
                for tb in range(NB):
                    k.op("pool", lambda e: e.tensor_tensor(out=G[:, jg, tsl(tb)], in0=sgt[:, tsl(tb)],
                                                           in1=accv[:, tsl(tb)], op=ALU.mult),
                         reads=[sgt_r[tb], accv_r[tb]], writes=[G_r[jg][tb]])
            if sb + 1 < len(subs):
                issue_v(sb + 1)
            if jb + nch < j1:
                continue
            ng = j1 - j0
            for dc in range(KC):
                for tb in range(NB):
                    b = k.bank()
                    for jg in range(ng):
                        k.op("pe", lambda e: e.matmul(out=ps[:, b, :], lhsT=wd[jg // 4][:, jg % 4, dc * 128:(dc + 1) * 128],
                                                      rhs=G[:, jg, tsl(tb)], start=(jg == 0), stop=(jg == ng - 1)),
                             reads=[wd_r[jg // 4], G_r[jg][tb]], writes=[pb[b]], inc=(jg == ng - 1))
                    k.op("dve", lambda e: e.tensor_tensor(out=hT[:, dc, tsl(tb)], in0=ps[:, b, :], in1=hT[:, dc, tsl(tb)],
                                                          op=ALU.add), reads=[pb[b], hT_r[dc][tb]], writes=[hT_r[dc][tb]])
            if gi + 1 < len(groups):
                issue_wd(gi + 1)
        if stages <= 4:
            break
        phase_end()

        arena["ptr"] = ARENA1
        if s + 1 < nseq:
            p1a_pre.append((load_w512(w_in[:, 0:512]), load_w512(w_in[:, 512:1024]), load_w512(w_in[:, 1024:1536])))
        sqf = [A("sqf%d_%d" % (s, i), [128, S], BF16) for i in range(2)]
        sqf_r = [k.res("sqf%d" % i) for i in range(2)]
        rstd3 = A("rstd3_%d" % s, [128, S], F32)
        rstd3_r = [k.res("rstd3_%d" % tb) for tb in range(NB)]

        def dst_o(kc, tb, g0):
            k.op("dve", lambda e: e.scalar_tensor_tensor(out=hT[:, kc, tsl(tb)], in0=hT[:, kc, tsl(tb)],
                                                         scalar=vcol(g0 + kc), in1=rstd3[:, tsl(tb)],
                                                         op0=ALU.mult, op1=ALU.mult),
                 reads=[hT_r[kc][tb], rstd3_r[tb], r_const], writes=[hT_r[kc][tb]])
            k.dma("sp", out=outT[s, kc * 128:(kc + 1) * 128, tsl(tb)], in_=hT[:, kc, tsl(tb)], sem=osem[kc],
                  reads=[hT_r[kc][tb]])

        norm_phase(hT, hT_r, V_GF, dst_o, sqf, sqf_r, rstd3, rstd3_r, kc_outer=True)
        phase_end()
    for o_ in osem + [gsem]:
        if o_.n:
            k.engs["sp"].wait_ge(o_.h, o_.n)
    return nc, dbg, k


def host_prep(inputs):
    f = np.float32
    x = np.asarray(inputs["x"], f)
    vecs = np.zeros((128, NVEC), f)

    def cols(v):
        v = np.asarray(v, f).reshape(-1)
        return v.reshape(-1, 128).T

    vecs[:, V_G1:V_G1 + 8] = cols(inputs["norm_mix_g"][0])
    vecs[:, V_G2:V_G2 + 8] = cols(inputs["norm_ffn_g"][0])
    vecs[:, V_GF:V_GF + 8] = cols(inputs["norm_final_g"])
    vecs[:, V_GATT:V_GATT + 4] = cols(inputs["att_out_g"][0])
    vecs[:, V_GML:V_GML + 4] = cols(inputs["mlstm_out_g"][0])
    mcw = np.asarray(inputs["mlstm_conv_w"][0], f)
    for j in range(4):
        vecs[:, V_MCW + j * 8:V_MCW + (j + 1) * 8] = cols(mcw[j])
    vecs[:, V_MCB:V_MCB + 8] = cols(inputs["mlstm_conv_b"][0])
    fcw = np.asarray(inputs["ffn_conv_w"][0], f)
    for j in range(3):
        vecs[:, V_FCW + j * 44:V_FCW + (j + 1) * 44] = cols(fcw[j])
    vecs[:, V_FCB:V_FCB + 44] = cols(inputs["ffn_conv_b"][0])
    vecs[:, V_BG:V_BG + 8] = np.asarray(inputs["b_gates"][0], f)[None, :]
    cstf = np.zeros((128, NCST), f)
    cstf[:, C_ID:C_ID + 128] = np.eye(128, dtype=f)
    cstf[:, C_TRI:C_TRI + 128] = np.triu(np.ones((128, 128), f))
    cstf[:, C_ONE:C_ONE + 128] = 1.0
    sel = np.zeros((128, 64, 128), f)
    for p in range(64):
        sel[p, p, :] = 1.0
    common = {
        "w_in": np.ascontiguousarray(inputs["w_in"][0], f),
        "w_out": np.ascontiguousarray(inputs["w_out"][0], f),
        "w_up": np.ascontiguousarray(inputs["w_up"][0], f),
        "w_down": np.ascontiguousarray(inputs["w_down"][0], f),
        "vecs": vecs, "cstf": cstf, "sel": sel.reshape(128, 64 * 128),
    }
    in_maps = []
    for c in range(8):
        m = dict(common)
        m["xT"] = np.ascontiguousarray(np.transpose(x[2 * c:2 * c + 2], (0, 2, 1)))
        in_maps.append(m)
    return in_maps


def kernel(**inputs):
    nc, _, _ = build()
    in_maps = host_prep(inputs)
    res = run_bass_kernel_spmd(nc, in_maps, core_ids=list(range(8)))
    out = np.empty((16, S, D), np.float32)
    for c in range(8):
        o = np.asarray(res.results[c]["outT"])
        out[2 * c:2 * c + 2] = np.transpose(o, (0, 2, 1))
    return out
```
